# Optimizing a Trainium2 kernel written in Bass

```python
import math
import jax
import jax.numpy as jnp
from jax import lax
import numpy as np

D_MODEL = 1024
BATCH = 8
SEQ = 4096
DEPTH = 2

GRID_W = 64
CTX_LEN = 256
HEAD_DIM = 64
D_HY = 3 * D_MODEL // 8
D_NA = 3 * D_MODEL // 8
D_GA = D_MODEL - D_HY - D_NA
N_NA = D_NA // HEAD_DIM
N_GA = D_GA // HEAD_DIM
KH_MAX = 8
KW = 16
HY_EMB = 33
HY_ORDER = 64
HY_INNER = 2
HY_FAST_DECAY = 0.3
HY_SLOW_DECAY = 1.5
HY_TARGET = 1e-2
SHORT_K = 3
D_FF = 2816
N_EXPERTS = 8
TOP_K = 2
D_FF_E = 3584
ROPE_BASE = 10000.0
Q_BLOCK = 128
N_MOD = 6
EPS = 1e-6

kernel_name = "hybrid_hyena_natten_global_moe_dit"


def rms_norm(x, g):
    xf = x.astype(jnp.float32)
    y = xf * lax.rsqrt(jnp.mean(xf * xf, axis=-1, keepdims=True) + EPS)
    return (y * g.astype(jnp.float32)).astype(x.dtype)


def modulate(x, g, shift, scale):
    return rms_norm(x, g) * (1.0 + scale) + shift


def split_proj(p):
    B, L, _ = p.shape
    hy = p[..., :3 * D_HY]
    na = p[..., 3 * D_HY:3 * (D_HY + D_NA)].reshape(B, L, 3, N_NA, HEAD_DIM)
    ga = p[..., 3 * (D_HY + D_NA):].reshape(B, L, 3, N_GA, HEAD_DIM)
    return hy, na[:, :, 0], na[:, :, 1], na[:, :, 2], ga[:, :, 0], ga[:, :, 1], ga[:, :, 2]


def short_conv(u, w, b):
    L = u.shape[1]
    pad = SHORT_K // 2
    up = jnp.pad(u, ((0, 0), (pad, SHORT_K - 1 - pad), (0, 0)))
    y = b
    for j in range(SHORT_K):
        y = y + up[:, j:j + L] * w[j]
    return y


def hyena_filter(L, w1, b1, freq, w2, b2, w3):
    f32 = jnp.float32
    bands = (HY_EMB - 1) // 2
    t = jnp.linspace(0.0, 1.0, L, dtype=f32)[:, None]
    ang = 2.0 * math.pi * jnp.arange(L, dtype=f32)[:, None] / L
    fr = jnp.linspace(1e-4, bands - 1, bands, dtype=f32)[None, :]
    z = jnp.concatenate([t, jnp.cos(fr * ang), -jnp.sin(fr * ang)], axis=-1)
    w = freq.astype(f32)
    h = jnp.sin(w * (z @ w1.astype(f32) + b1.astype(f32)))
    for i in range(HY_INNER):
        h = jnp.sin(w * (h @ w2[i].astype(f32) + b2[i].astype(f32)))
    h = h @ w3.astype(f32)
    deltas = jnp.linspace(math.log(HY_TARGET) / HY_SLOW_DECAY,
                          math.log(HY_TARGET) / HY_FAST_DECAY, D_HY, dtype=f32)
    deltas = jnp.concatenate([deltas, deltas])
    return h * jnp.exp(-t * jnp.abs(deltas))


def bidir_long_conv(u, h, skip):
    L, C = u.shape[1], u.shape[2]
    h_fwd, h_bwd = h[:, :C], h[:, C:]
    k_circ = jnp.concatenate([h_fwd, jnp.zeros((1, C), jnp.float32), h_bwd[1:][::-1]], axis=0)
    uf = jnp.fft.rfft(u.astype(jnp.float32), n=2 * L, axis=1)
    kf = jnp.fft.rfft(k_circ, n=2 * L, axis=0)
    y = jnp.fft.irfft(uf * kf[None], n=2 * L, axis=1)[:, :L]
    return (y + u.astype(jnp.float32) * skip.astype(jnp.float32)).astype(u.dtype)


def hyena_mixer(p, conv_w, conv_b, filt, skip):
    u = short_conv(p, conv_w, conv_b)
    x0, x1, v = jnp.split(u, 3, axis=-1)
    v = bidir_long_conv(v * x1, filt, skip)
    return v * x0


def context_attention(q, k, v):
    B, Lc, H, Dh = q.shape
    s = jnp.einsum('bqhd,bkhd->bhqk', q, k).astype(jnp.float32) * (Dh ** -0.5)
    p = jax.nn.softmax(s, axis=-1).astype(v.dtype)
    return jnp.einsum('bhqk,bkhd->bqhd', p, v).reshape(B, Lc, H * Dh)


def neighbourhood_attention(q, k, v, k_ctx, v_ctx, rpb):
    B, S, H, Dh = q.shape
    rows = S // GRID_W
    kh = min(KH_MAX, rows)
    r = jnp.arange(rows)
    cidx = jnp.arange(GRID_W)
    row_idx = jnp.clip(r - kh // 2, 0, rows - kh)[:, None] + jnp.arange(kh)[None, :]
    col_start = jnp.clip(cidx - KW // 2, 0, GRID_W - KW)
    col_mask = (cidx[None, :] >= col_start[:, None]) & (cidx[None, :] < col_start[:, None] + KW)
    qg = q.reshape(B, rows, GRID_W, H, Dh)
    kb = k.reshape(B, rows, GRID_W, H, Dh)[:, row_idx]
    vb = v.reshape(B, rows, GRID_W, H, Dh)[:, row_idx]
    scale = Dh ** -0.5
    s_lat = jnp.einsum('brqhd,brkwhd->bhrqkw', qg, kb).astype(jnp.float32) * scale
    dr = row_idx - r[:, None] + (KH_MAX - 1)
    dc = jnp.clip(cidx[None, :] - cidx[:, None], -(KW - 1), KW - 1) + (KW - 1)
    bias = rpb[:, dr[:, None, :, None], dc[None, :, None, :]]
    s_lat = jnp.where(col_mask[:, None, :], s_lat + bias.astype(jnp.float32), -jnp.inf)
    s_ctx = jnp.einsum('brqhd,bchd->bhrqc', qg, k_ctx).astype(jnp.float32) * scale
    n_lat = kh * GRID_W
    s = jnp.concatenate([s_lat.reshape(B, H, rows, GRID_W, n_lat), s_ctx], axis=-1)
    p = jax.nn.softmax(s, axis=-1).astype(v.dtype)
    p_lat = p[..., :n_lat].reshape(B, H, rows, GRID_W, kh, GRID_W)
    o = (jnp.einsum('bhrqkw,brkwhd->brqhd', p_lat, vb)
         + jnp.einsum('bhrqc,bchd->brqhd', p[..., n_lat:], v_ctx))
    return o.reshape(B, S, H * Dh)


def global_attention(q, k, v, k_ctx, v_ctx):
    B, S, H, Dh = q.shape
    nb = S // Q_BLOCK
    scale = Dh ** -0.5
    qb = q.reshape(B, nb, Q_BLOCK, H, Dh).transpose(1, 0, 2, 3, 4)

    def block(qi):
        s = jnp.concatenate([jnp.einsum('bqhd,bkhd->bhqk', qi, k),
                             jnp.einsum('bqhd,bchd->bhqc', qi, k_ctx)], axis=-1).astype(jnp.float32) * scale
        p = jax.nn.softmax(s, axis=-1).astype(v.dtype)
        return (jnp.einsum('bhqk,bkhd->bqhd', p[..., :S], v)
                + jnp.einsum('bhqc,bchd->bqhd', p[..., S:], v_ctx))

    o = lax.map(block, qb)
    return o.transpose(1, 0, 2, 3, 4).reshape(B, S, H * Dh)


def rope_1d(x, pos):
    n = x.shape[-1] // 2
    inv = jnp.power(ROPE_BASE, -jnp.arange(n, dtype=jnp.float32) / n)
    ang = pos[:, None] * inv[None, :]
    cos = jnp.cos(ang)[None, :, None, :]
    sin = jnp.sin(ang)[None, :, None, :]
    x1, x2 = x[..., :n], x[..., n:]
    return jnp.concatenate([x1 * cos - x2 * sin, x1 * sin + x2 * cos], axis=-1)


def axial_rope(x, pos_row, pos_col):
    xf = x.astype(jnp.float32)
    half = x.shape[-1] // 2
    return jnp.concatenate([rope_1d(xf[..., :half], pos_row),
                            rope_1d(xf[..., half:], pos_col)], axis=-1).astype(x.dtype)


def swiglu(h, w_gate, w_up, w_down):
    return (jax.nn.silu(h @ w_gate) * (h @ w_up)) @ w_down


def moe_ffn(h, w_router, w_gate, w_up, w_down):
    logits = (h @ w_router).astype(jnp.float32)
    top_v, top_i = lax.top_k(logits, TOP_K)
    top_w = jax.nn.softmax(top_v, axis=-1)
    combine = jnp.sum(jax.nn.one_hot(top_i, N_EXPERTS, dtype=jnp.float32) * top_w[..., None], axis=-2)
    combine = combine.astype(h.dtype)
    out = jnp.zeros_like(h)
    for e in range(N_EXPERTS):
        out = out + combine[..., e:e + 1] * swiglu(h, w_gate[e], w_up[e], w_down[e])
    return out


def setup_inputs(seed: int = 0) -> dict:
    key = jax.random.key(seed)
    ks = iter(jax.random.split(key, 40))
    f32 = jnp.float32

    def nrm(shape, scale):
        return jax.random.normal(next(ks), shape, f32) * scale

    def gain(shape):
        return 1.0 + nrm(shape, 0.02)

    L = DEPTH
    nd = (DEPTH + 1) // 2
    nm = DEPTH // 2
    return {
        "x": nrm((BATCH, SEQ, D_MODEL), 1.0),
        "c": nrm((BATCH, D_MODEL), 1.0),
        "ctx": nrm((BATCH, CTX_LEN, D_MODEL), 1.0),
        "c_ctx": nrm((D_MODEL,), 1.0),
        "w_mod": nrm((L, D_MODEL, N_MOD * D_MODEL), 0.3 * D_MODEL ** -0.5),
        "b_mod": nrm((L, N_MOD * D_MODEL), 0.02),
        "norm_mix": gain((L, D_MODEL)),
        "norm_ffn": gain((L, D_MODEL)),
        "w_in": nrm((L, D_MODEL, 3 * D_MODEL), D_MODEL ** -0.5),
        "hy_conv_w": nrm((L, SHORT_K, 3 * D_HY), SHORT_K ** -0.5),
        "hy_conv_b": nrm((L, 3 * D_HY), 0.02),
        "hy_w1": nrm((L, HY_EMB, HY_ORDER), HY_EMB ** -0.5),
        "hy_b1": nrm((L, HY_ORDER), 0.2),
        "hy_freq": gain((L, HY_ORDER)),
        "hy_w2": nrm((L, HY_INNER, HY_ORDER, HY_ORDER), HY_ORDER ** -0.5),
        "hy_b2": nrm((L, HY_INNER, HY_ORDER), 0.2),
        "hy_w3": nrm((L, HY_ORDER, 2 * D_HY), HY_ORDER ** -0.5),
        "hy_skip": nrm((L, D_HY), 1.0),
        "na_q_norm": gain((L, HEAD_DIM)),
        "na_k_norm": gain((L, HEAD_DIM)),
        "na_rpb": nrm((L, N_NA, 2 * KH_MAX - 1, 2 * KW - 1), 0.02),
        "ga_q_norm": gain((L, HEAD_DIM)),
        "ga_k_norm": gain((L, HEAD_DIM)),
        "out_norm_hy": gain((L, D_HY)),
        "out_norm_na": gain((L, D_NA)),
        "out_norm_ga": gain((L, D_GA)),
        "w_out": nrm((L, D_MODEL, D_MODEL), D_MODEL ** -0.5),
        "ffn_w_gate": nrm((nd, D_MODEL, D_FF), D_MODEL ** -0.5),
        "ffn_w_up": nrm((nd, D_MODEL, D_FF), D_MODEL ** -0.5),
        "ffn_w_down": nrm((nd, D_FF, D_MODEL), D_FF ** -0.5),
        "moe_router": nrm((nm, D_MODEL, N_EXPERTS), D_MODEL ** -0.5),
        "moe_w_gate": nrm((nm, N_EXPERTS, D_MODEL, D_FF_E), D_MODEL ** -0.5),
        "moe_w_up": nrm((nm, N_EXPERTS, D_MODEL, D_FF_E), D_MODEL ** -0.5),
        "moe_w_down": nrm((nm, N_EXPERTS, D_FF_E, D_MODEL), D_FF_E ** -0.5),
    }


def reference(x, c, ctx, c_ctx, w_mod, b_mod, norm_mix, norm_ffn, w_in,
              hy_conv_w, hy_conv_b, hy_w1, hy_b1, hy_freq, hy_w2, hy_b2, hy_w3, hy_skip,
              na_q_norm, na_k_norm, na_rpb, ga_q_norm, ga_k_norm,
              out_norm_hy, out_norm_na, out_norm_ga, w_out,
              ffn_w_gate, ffn_w_up, ffn_w_down,
              moe_router, moe_w_gate, moe_w_up, moe_w_down):
    S = x.shape[1]
    Lc = ctx.shape[1]
    t = jnp.arange(S)
    pos_row = (t // GRID_W).astype(jnp.float32)
    pos_col = (t % GRID_W).astype(jnp.float32)

    for l in range(DEPTH):
        last = l == DEPTH - 1
        mod_x = jnp.split((jax.nn.silu(c) @ w_mod[l] + b_mod[l])[:, None, :], N_MOD, axis=-1)
        mod_c = jnp.split((jax.nn.silu(c_ctx) @ w_mod[l] + b_mod[l])[None, None, :], N_MOD, axis=-1)

        hx = modulate(x, norm_mix[l], mod_x[0], mod_x[1])
        hc = modulate(ctx, norm_mix[l], mod_c[0], mod_c[1])
        hy_x, qn_x, kn_x, vn_x, qg_x, kg_x, vg_x = split_proj(hx @ w_in[l])
        hy_c, qn_c, kn_c, vn_c, qg_c, kg_c, vg_c = split_proj(hc @ w_in[l])
        qn_x, kn_x = rms_norm(qn_x, na_q_norm[l]), rms_norm(kn_x, na_k_norm[l])
        qn_c, kn_c = rms_norm(qn_c, na_q_norm[l]), rms_norm(kn_c, na_k_norm[l])
        qg_x, kg_x = rms_norm(qg_x, ga_q_norm[l]), rms_norm(kg_x, ga_k_norm[l])
        qg_c, kg_c = rms_norm(qg_c, ga_q_norm[l]), rms_norm(kg_c, ga_k_norm[l])
        qg_x = axial_rope(qg_x, pos_row, pos_col)
        kg_x = axial_rope(kg_x, pos_row, pos_col)

        filt_x = hyena_filter(S, hy_w1[l], hy_b1[l], hy_freq[l], hy_w2[l], hy_b2[l], hy_w3[l])
        y_hy = hyena_mixer(hy_x, hy_conv_w[l], hy_conv_b[l], filt_x, hy_skip[l])
        y_na = neighbourhood_attention(qn_x, kn_x, vn_x, kn_c, vn_c, na_rpb[l])
        y_ga = global_attention(qg_x, kg_x, vg_x, kg_c, vg_c)
        y = jnp.concatenate([rms_norm(y_hy, out_norm_hy[l]), rms_norm(y_na, out_norm_na[l]),
                             rms_norm(y_ga, out_norm_ga[l])], axis=-1) @ w_out[l]
        x = x + mod_x[2] * y

        if not last:
            filt_c = hyena_filter(Lc, hy_w1[l], hy_b1[l], hy_freq[l], hy_w2[l], hy_b2[l], hy_w3[l])
            yc_hy = hyena_mixer(hy_c, hy_conv_w[l], hy_conv_b[l], filt_c, hy_skip[l])
            yc_na = context_attention(qn_c, kn_c, vn_c)
            yc_ga = context_attention(qg_c, kg_c, vg_c)
            yc = jnp.concatenate([rms_norm(yc_hy, out_norm_hy[l]), rms_norm(yc_na, out_norm_na[l]),
                                  rms_norm(yc_ga, out_norm_ga[l])], axis=-1) @ w_out[l]
            ctx = ctx + mod_c[2] * yc

        if l % 2 == 0:
            i = l // 2
            ffn = functools_partial_dense(ffn_w_gate[i], ffn_w_up[i], ffn_w_down[i])
        else:
            i = l // 2
            ffn = functools_partial_moe(moe_router[i], moe_w_gate[i], moe_w_up[i], moe_w_down[i])
        x = x + mod_x[5] * ffn(modulate(x, norm_ffn[l], mod_x[3], mod_x[4]))
        if not last:
            ctx = ctx + mod_c[5] * ffn(modulate(ctx, norm_ffn[l], mod_c[3], mod_c[4]))

    return x


def functools_partial_dense(w_gate, w_up, w_down):
    def f(h):
        return swiglu(h, w_gate, w_up, w_down)
    return f


def functools_partial_moe(w_router, w_gate, w_up, w_down):
    def f(h):
        return moe_ffn(h, w_router, w_gate, w_up, w_down)
    return f
```

```python
import math
from contextlib import ExitStack, contextmanager
import numpy as np
import ml_dtypes
import concourse.bass as bass
import concourse.mybir as mybir
from concourse.bass_utils import run_bass_kernel_spmd

F32 = mybir.dt.float32
BF16 = mybir.dt.bfloat16
AF = mybir.ActivationFunctionType
ALU = mybir.AluOpType
AX = mybir.AxisListType

D = 1024
KC = 8
S = 4096
LC = 256
NTOK = S + LC
GW = 64
DHY = 384
DFF = 2816
NE = 8
DFE = 3584
EPS = 1e-6
PI = math.pi
NTM = 23
NSLOT = NTM * 512
BIG = 16384.0
I32 = mybir.dt.int32


class Res:
    __slots__ = ("w", "r")

    def __init__(self):
        self.w = None
        self.r = {}


class Tl:
    def __init__(self, t):
        self.t = t
        self._res = {}

    def r(self, key=None):
        x = self._res.get(key)
        if x is None:
            x = self._res[key] = Res()
        return x

    def __getitem__(self, idx):
        return self.t[idx]


class KB:
    ENG = ("pe", "act", "dve", "pool", "sp")

    def __init__(self, nc, es, nslots=8):
        self.nc = nc
        self.es = es
        self.eng = {"pe": nc.tensor, "act": nc.scalar, "dve": nc.vector, "pool": nc.gpsimd, "sp": nc.sync}
        self.semh = {}
        self.cnt = {}
        for e in self.ENG:
            self.semh[e] = es.enter_context(nc.semaphore("c_" + e))
            self.cnt[e] = 0
        self.seen = {e: {} for e in self.ENG}
        self.slots = {}
        self.slotpos = {}
        for q in ("sp", "pool"):
            self.slots[q] = []
            for i in range(nslots):
                k = ("d", q, i)
                self.semh[k] = es.enter_context(nc.semaphore("d_%s%d" % (q, i)))
                self.cnt[k] = 0
                self.slots[q].append(k)
            self.slotpos[q] = 0
        self.ninst = 0
        self.uid = 0

    def sb(self, name, shape, dt):
        self.uid += 1
        return Tl(self.es.enter_context(self.nc.sbuf_tensor("%s_%d" % (name, self.uid), list(shape), dt)))

    def ps(self, name, shape, dt):
        return Tl(self.es.enter_context(self.nc.psum_tensor(name, list(shape), dt)))

    def dram(self, name, shape, dt, kind):
        return Tl(self.nc.dram_tensor(name, list(shape), dt, kind=kind).ap())

    @contextmanager
    def scope(self):
        old = self.es
        with ExitStack() as s:
            self.es = s
            try:
                yield
            finally:
                self.barrier()
                self.es = old

    def barrier(self):
        for e in self.ENG:
            seen = self.seen[e]
            for k, v in self.cnt.items():
                if k != e and v > 0 and seen.get(k, 0) < v:
                    self.eng[e].wait_ge(self.semh[k], v)
                    seen[k] = v
                    self.ninst += 1

    def _deps(self, e, reads, writes):
        raw = {}
        oth = {}
        for r in reads:
            ev = r.w
            if ev is not None and raw.get(ev[0], 0) < ev[1]:
                raw[ev[0]] = ev[1]
        for w in writes:
            ev = w.w
            if ev is not None and oth.get(ev[0], 0) < ev[1]:
                oth[ev[0]] = ev[1]
            for k, v in w.r.items():
                if oth.get(k, 0) < v:
                    oth[k] = v
        for k, v in oth.items():
            if k == e and e == "pe":
                continue
            if raw.get(k, 0) < v:
                raw[k] = v
        seen = self.seen[e]
        eng = self.eng[e]
        for k, v in raw.items():
            if seen.get(k, 0) < v:
                eng.wait_ge(self.semh[k], v)
                seen[k] = v
                self.ninst += 1

    def _mark(self, ev, reads, writes):
        k, v = ev
        for r in reads:
            if r.r.get(k, 0) < v:
                r.r[k] = v
        for w in writes:
            w.w = ev
            w.r = {}

    def op(self, e, fn, reads=(), writes=()):
        self._deps(e, reads, writes)
        inst = fn(self.eng[e])
        self.cnt[e] += 1
        inst.then_inc(self.semh[e], 1)
        self.ninst += 1
        self._mark((e, self.cnt[e]), reads, writes)
        return inst

    def dma(self, q, out, in_, reads=(), writes=(), **kw):
        k = self.slots[q][self.slotpos[q]]
        self.slotpos[q] = (self.slotpos[q] + 1) % len(self.slots[q])
        self._deps(q, reads, writes)
        seen = self.seen[q]
        if seen.get(k, 0) < self.cnt[k]:
            self.eng[q].wait_ge(self.semh[k], self.cnt[k])
            seen[k] = self.cnt[k]
        inst = self.eng[q].dma_start(out=out, in_=in_, **kw)
        self.cnt[k] += 16
        inst.then_inc(self.semh[k], 16)
        self.ninst += 1
        self._mark((k, self.cnt[k]), reads, writes)
        return inst

    def idma(self, out, out_off, in_, in_off, reads=(), writes=()):
        q = "pool"
        k = self.slots[q][self.slotpos[q]]
        self.slotpos[q] = (self.slotpos[q] + 1) % len(self.slots[q])
        self._deps(q, reads, writes)
        seen = self.seen[q]
        if seen.get(k, 0) < self.cnt[k]:
            self.eng[q].wait_ge(self.semh[k], self.cnt[k])
            seen[k] = self.cnt[k]
        inst = self.nc.gpsimd.indirect_dma_start(out, out_off, in_, in_off)
        self.cnt[k] += 16
        inst.then_inc(self.semh[k], 16)
        self.ninst += 1
        self._mark((k, self.cnt[k]), reads, writes)
        return inst

    def finish(self):
        sp = self.eng["sp"]
        for k, v in self.cnt.items():
            if v > 0 and k != "sp" and self.seen["sp"].get(k, 0) < v:
                sp.wait_ge(self.semh[k], v)


def _bf(a):
    return np.ascontiguousarray(a.astype(ml_dtypes.bfloat16))


def _pk(v, nchunk):
    return np.ascontiguousarray(np.asarray(v, np.float32).reshape(nchunk, 128).T)


_CONST_CACHE = {}


def _dft_consts(L):
    t = np.arange(L, dtype=np.int64)[:, None]
    f = np.arange(L, dtype=np.int64)[None, :]
    m = ((2 * f + 1) * t) % (4 * L)
    ang = m.astype(np.float64) * (np.pi / (2 * L))
    out = {}
    nt = L // 128
    W = min(512, L)
    for nm, M in (("C", np.cos(ang)), ("S", np.sin(ang))):
        Mb = M.astype(np.float32).astype(ml_dtypes.bfloat16)
        out["FW" + nm] = np.ascontiguousarray(Mb.reshape(nt, 128, nt, 128).transpose(2, 1, 0, 3))
        out["IV" + nm] = np.ascontiguousarray(Mb.reshape(L // W, W, nt, 128).transpose(0, 3, 2, 1))
    return out


def _hy_pos(L):
    bands = 16
    t = np.linspace(0.0, 1.0, L, dtype=np.float32)[:, None]
    ang = (2.0 * np.float32(math.pi) * np.arange(L, dtype=np.float32)[:, None] / np.float32(L)).astype(np.float32)
    fr = np.linspace(1e-4, bands - 1, bands, dtype=np.float32)[None, :]
    z = np.concatenate([t, np.cos(fr * ang), -np.sin(fr * ang)], axis=-1).astype(np.float32)
    return np.ascontiguousarray(z.T), np.ascontiguousarray((-t[:, 0]).reshape(L // 128, 128).T)


def _consts():
    if _CONST_CACHE:
        return _CONST_CACHE
    c = {}
    c["ident"] = np.eye(128, dtype=np.float32)
    c["ones_b"] = _bf(np.ones((128, 128), np.float32))
    blk = np.zeros((128, 128), np.float32)
    blk[:64, :64] = 1
    blk[64:, 64:] = 1
    c["blk_b"] = _bf(blk)
    rm = np.zeros((128, 128), np.float32)
    for m in range(128):
        d = m % 32
        if d < 16:
            rm[m + 16, m] = -1.0
        else:
            rm[m - 16, m] = 1.0
    c["rm_b"] = _bf(rm)
    sel = np.zeros((8, 8, 128), np.float32)
    for e in range(8):
        sel[e, e, :] = 1.0
    c["sel"] = sel.reshape(8, 1024)
    tok = np.arange(S)
    pos_row = (tok // GW).astype(np.float32)
    pos_col = (tok % GW).astype(np.float32)
    inv = np.power(np.float32(10000.0), -np.arange(16, dtype=np.float32) / np.float32(16)).astype(np.float32)
    cosT = np.zeros((128, S), np.float32)
    sinT = np.zeros((128, S), np.float32)
    for p in range(128):
        d = p % 64
        pos = pos_row if d < 32 else pos_col
        a = pos * inv[d % 16]
        cosT[p] = np.cos(a.astype(np.float32))
        sinT[p] = np.sin(a.astype(np.float32))
    c["cosT"] = _bf(cosT)
    c["sinT"] = _bf(sinT)
    for L in (S, LC):
        zp, negt = _hy_pos(L)
        c["zpos%d" % L] = zp
        c["negt%d" % L] = negt
        for k, v in _dft_consts(L).items():
            c["%s%d" % (k, L)] = v
    deltas = np.linspace(math.log(1e-2) / 1.5, math.log(1e-2) / 0.3, DHY, dtype=np.float32)
    c["dabs"] = np.ascontiguousarray(np.tile(np.abs(deltas)[None, :], (128, 1)).astype(np.float32))
    cidx = np.arange(GW)
    cs = np.clip(cidx - 8, 0, GW - 16)
    cm = ((cidx[None, :] >= cs[:, None]) & (cidx[None, :] < cs[:, None] + 16))
    c["colmask"] = np.ascontiguousarray(np.tile(cm.T.astype(np.float32), (2, 1)))
    ut = np.triu(np.ones((128, 128), np.float32), 1)
    c["ut_b"] = _bf(ut)
    c["thr"] = np.ascontiguousarray(np.tile((512.0 * np.arange(8, dtype=np.float32))[None, None, :], (128, 8, 1)).reshape(128, 64))
    c["tidx"] = np.ascontiguousarray(np.tile(np.arange(NTM, dtype=np.float32)[None, :, None], (128, 1, 8)).reshape(128, NTM * 8))
    p = np.arange(128, dtype=np.float32)[:, None]
    kq = np.arange(32, dtype=np.float32)[None, :]
    c["cgu"] = np.ascontiguousarray(((kq // 4) * 128 + p) * 4 + (kq % 4)).astype(np.float32)
    c["cdn"] = np.ascontiguousarray(np.arange(28, dtype=np.float32)[None, :] * 128 + p).astype(np.float32)
    _CONST_CACHE.update(c)
    return c


def _na_variant(r):
    w0 = min(max(r - 4, 0), 56)
    return (w0 % 2, w0 - r)


NA_VARIANTS = sorted(set(_na_variant(r) for r in range(64)))


def build_program(dbg=(), nlayers=2, stop=None):
    nc = bass.Bass("TRN2", target_bir_lowering=False)
    es = ExitStack()
    kb = KB(nc, es, nslots=8)
    cst = _consts()
    din = {}

    def inp(name, shape, dt=F32):
        din[name] = kb.dram(name, shape, dt, "ExternalInput")
        return din[name]

    x_d = inp("x", [S, D])
    ctx_d = inp("ctx", [LC, D])
    cpk_d = inp("c_pk", [128, 16])
    wmod_d = inp("w_mod", [2, D, 6 * D])
    bmod_d = inp("b_mod_pk", [2, 128, 48])
    nrm_d = inp("norm_pk", [2, 128, 16])
    win_d = inp("w_in", [2, D, 3 * D])
    cw_d = inp("conv_pk", [2, 128, 36])
    hyw1_d = inp("hy_w1", [2, 33, 64])
    hyv_d = inp("hy_vec", [2, 64, 4])
    hyw2_d = inp("hy_w2", [2, 2, 64, 64])
    hyw3_d = inp("hy_w3", [2, 64, 768])
    hysk_d = inp("hy_skip", [2, 1, 384])
    qkn_d = inp("qkn_pk", [2, 128, 4])
    rpb_d = inp("rpbT", [2, 64, 90, 64])
    onrm_d = inp("onorm_pk", [2, 128, 8])
    wout_d = inp("w_out", [2, D, D])
    fg_d = inp("ffn_w_gate", [1, D, DFF])
    fu_d = inp("ffn_w_up", [1, D, DFF])
    fd_d = inp("ffn_w_down", [1, DFF, D])
    mr_d = inp("moe_router", [1, D, NE])
    mg_d = inp("moe_w_gate", [1, NE, D, DFE])
    mu_d = inp("moe_w_up", [1, NE, D, DFE])
    md_d = inp("moe_w_down", [1, NE, DFE, D])
    cd = {}
    for k, v in cst.items():
        cd[k] = inp("k_" + k, list(v.shape), BF16 if v.dtype == ml_dtypes.bfloat16 else F32)
    out_d = kb.dram("out", [S, D], F32, "ExternalOutput")
    dbg_d = {}
    xT_d = kb.dram("xT_scr", [D, NTOK], F32, "Internal")
    x0c_d = kb.dram("x0c_scr", [128, 3, NTOK], BF16, "Internal")
    h2_d = kb.dram("h2_scr", [128, 8, NTOK], BF16, "Internal")
    h2tm_d = kb.dram("h2tm_scr", [S, D], BF16, "Internal")
    xtm_d = kb.dram("xtm_scr", [S, D], F32, "Internal")
    hslot_d = kb.dram("hslot_scr", [NSLOT, D], BF16, "Internal")
    yslot_d = kb.dram("yslot_scr", [NSLOT, D], F32, "Internal")
    g5_d = kb.dram("g5_scr", [1, D], F32, "Internal")
    xTv = xT_d.t.rearrange("(k p) t -> p k t", p=128)

    def dbg_dump(name, ap, shape, dt):
        if name in dbg:
            dbg_d[name] = kb.dram("dbg_" + name, shape, dt, "ExternalOutput")
            kb.barrier()
            kb.dma("sp", dbg_d[name].t, ap)
            kb.barrier()

    PB = [kb.ps("bank%d" % i, [128, 512], F32) for i in range(8)]

    def mm(out, lhsT, rhs, start, stop, reads, writes):
        kb.op("pe", lambda e: e.matmul(out, lhsT, rhs, start=start, stop=stop), reads, writes)

    def act(out, in_, func, reads, writes, **kw):
        kb.op("act", lambda e: e.activation(out=out, in_=in_, func=func, **kw), reads, writes)

    def tt(eng, out, in0, in1, op, reads, writes):
        kb.op(eng, lambda e: e.tensor_tensor(out=out, in0=in0, in1=in1, op=op), reads, writes)

    def ts(eng, out, in0, s1, s2, op0, op1, reads, writes):
        if op1 is None:
            kb.op(eng, lambda e: e.tensor_scalar(out=out, in0=in0, scalar1=s1, scalar2=None, op0=op0), reads, writes)
        else:
            kb.op(eng, lambda e: e.tensor_scalar(out=out, in0=in0, scalar1=s1, scalar2=s2, op0=op0, op1=op1), reads, writes)

    def stt(out, in0, scalar, in1, op0, op1, reads, writes):
        kb.op("dve", lambda e: e.scalar_tensor_tensor(out=out, in0=in0, scalar=scalar, in1=in1, op0=op0, op1=op1), reads, writes)

    def cp(eng, out, in_, reads, writes):
        if eng == "act":
            act(out, in_, AF.Copy, reads, writes)
        else:
            kb.op(eng, lambda e: e.tensor_copy(out=out, in_=in_), reads, writes)

    def recip(out, in_, reads, writes):
        kb.op("dve", lambda e: e.reciprocal(out=out, in_=in_), reads, writes)

    ident = kb.sb("ident", [128, 128], F32)
    kb.dma("sp", ident[:], cd["ident"][:, :], writes=[ident.r()])
    ident_b = kb.sb("identb", [128, 128], BF16)
    kb.dma("pool", ident_b[:], cd["ident"][:, :], writes=[ident_b.r()])
    ones_b = kb.sb("ones_b", [128, 128], BF16)
    kb.dma("sp", ones_b[:], cd["ones_b"][:, :], writes=[ones_b.r()])
    blk_b = kb.sb("blk_b", [128, 128], BF16)
    kb.dma("sp", blk_b[:], cd["blk_b"][:, :], writes=[blk_b.r()])
    rm_b = kb.sb("rm_b", [128, 128], BF16)
    kb.dma("sp", rm_b[:], cd["rm_b"][:, :], writes=[rm_b.r()])
    cpk = kb.sb("cpk", [128, 16], F32)
    kb.dma("sp", cpk[:], cpk_d[:, :], writes=[cpk.r()])
    sc_b = kb.sb("sc_b", [128, 8, 2], BF16)
    act(sc_b[:, :, 0], cpk[:, 0:8], AF.Silu, [cpk.r()], [sc_b.r()])
    act(sc_b[:, :, 1], cpk[:, 8:16], AF.Silu, [cpk.r()], [sc_b.r()])
    modT = kb.sb("modT", [128, 48, 2], F32)
    aff = kb.sb("aff", [128, 6, 8, 2], F32)
    prm = kb.sb("prm", [128, 2, 72], F32)
    bmod = kb.sb("bmod", [128, 2, 48], F32)
    wr32 = kb.sb("wr32", [128, 8, 8], F32)
    kb.dma("sp", wr32[:], mr_d.t[0].rearrange("(k p) n -> p k n", p=128), writes=[wr32.r()])
    lg = kb.sb("lg", [128, 40], F32)
    rt = {}
    mask_d = kb.dram("mask_scr", [128, 256], BF16, "Internal")
    comb_d = kb.dram("comb_scr", [128, 256], F32, "Internal")
    with kb.scope():
        zt = kb.sb("zt", [128, 4096], BF16)
        kb.op("pool", lambda e: e.memset(zt[:], 0.0), [], [zt.r()])
        hv = hslot_d.t.rearrange("(p a) n -> p (a n)", p=128)
        for i in range(NSLOT * D // 128 // 4096):
            kb.dma("sp", hv[:, i * 4096:(i + 1) * 4096], zt[:], reads=[zt.r()], writes=[hslot_d.r()])
    for l in range(2):
        kb.dma("sp", prm[:, l, 0:16], nrm_d[l, :, :], writes=[prm.r()])
        kb.dma("sp", prm[:, l, 16:52], cw_d[l, :, :], writes=[prm.r()])
        kb.dma("sp", prm[:, l, 52:56], qkn_d[l, :, :], writes=[prm.r()])
        kb.dma("sp", prm[:, l, 56:64], onrm_d[l, :, :], writes=[prm.r()])
        kb.dma("sp", bmod[:, l, :], bmod_d[l, :, :], writes=[bmod.r()])
    for l in range(2):
        ts("dve", prm[:, l, 64:65], prm[:, l, 52:53], 0.125, None, ALU.mult, None, [prm.r()], [prm.r()])
        ts("dve", prm[:, l, 65:66], prm[:, l, 54:55], 0.125, None, ALU.mult, None, [prm.r()], [prm.r()])

    TILES = [(i * 512, 512, False) for i in range(8)] + [(S, LC, True)]

    def mod_phase(l):
        with kb.scope():
            wm = [kb.sb("wm", [128, 8, 512], BF16) for _ in range(2)]
            bank = PB[0]
            src = wmod_d.t[l].rearrange("(k p) n -> p k n", p=128)
            for cb in range(12):
                w = wm[cb % 2]
                kb.dma("pool", w[:], src[:, :, cb * 512:(cb + 1) * 512], writes=[w.r()])
                for j in range(4):
                    oc = cb * 4 + j
                    for k in range(KC):
                        mm(bank[:, oc * 2:oc * 2 + 2], w[:, k, j * 128:(j + 1) * 128], sc_b[:, k, :], k == 0, k == KC - 1,
                           [w.r(), sc_b.r()], [bank.r()])
            tt("dve", modT[:], bank[:, 0:96].rearrange("p (a b) -> p a b", b=2),
               bmod[:, l, :].unsqueeze(2).to_broadcast([128, 48, 2]), ALU.add, [bank.r(), bmod.r()], [modT.r()])
            for (dst, gi, mi) in ((0, 0, 1), (3, 8, 4)):
                ts("dve", aff[:, dst], modT[:, mi * 8:(mi + 1) * 8, :], 1.0, None, ALU.add, None, [modT.r()], [aff.r()])
                tt("dve", aff[:, dst], aff[:, dst], prm[:, l, gi:gi + 8].unsqueeze(2).to_broadcast([128, 8, 2]), ALU.mult,
                   [aff.r(), prm.r()], [aff.r()])
            for (dst, mi) in ((1, 0), (2, 2), (4, 3), (5, 5)):
                cp("dve", aff[:, dst], modT[:, mi * 8:(mi + 1) * 8, :], [modT.r()], [aff.r()])

    def rms_tile(w, n_feat, chunks, pb, tmp):
        sq = tmp["sq"]
        n = len(chunks)
        for i, (ap, rd) in enumerate(chunks):
            act(sq[:, i % 2, :w], ap, AF.Square, rd, [sq.r(i % 2)])
            mm(pb[:, :w], ones_b[:], sq[:, i % 2, :w], i == 0, i == n - 1, [ones_b.r(), sq.r(i % 2)], [pb.r()])
        act(tmp["rt"][:, :w], pb[:, :w], AF.Ln, [pb.r()], [tmp["rt"].r()], scale=1.0 / n_feat, bias=EPS)
        act(tmp["R"][:, :w], tmp["rt"][:, :w], AF.Exp, [tmp["rt"].r()], [tmp["R"].r()], scale=-0.5)

    def norm_tile(l, sub, xt, ti, dst, tmp, h32=None):
        t0, w, isc = TILES[ti]
        wh = 1 if isc else 0
        ai, bi = (0, 1) if sub == 0 else (3, 4)
        rms_tile(w, D, [(xt[:, k, :w], [xt.r()]) for k in range(KC)], PB[7], tmp)
        for k in range(KC):
            u = tmp["u"]
            stt(u[:, k % 2, :w], xt[:, k, :w], aff[:, ai, k, wh:wh + 1], tmp["R"][:, :w], ALU.mult, ALU.mult,
                [xt.r(), aff.r(), tmp["R"].r()], [u.r(k % 2)])
            oap, ores = dst(k)
            act(oap, u[:, k % 2, :w], AF.Identity, [u.r(k % 2), aff.r()], ores, bias=aff[:, bi, k, wh:wh + 1])
            if h32 is not None:
                ts("pool", h32[:, k, :w], u[:, k % 2, :w], 1.0, aff[:, bi, k, wh:wh + 1], ALU.mult, ALU.add, [u.r(k % 2), aff.r()], [h32.r()])

    def norm_tmp():
        return {"sq": kb.sb("sq", [128, 2, 512], BF16), "rt": kb.sb("rt", [128, 512], F32), "R": kb.sb("R", [128, 512], F32),
                "u": kb.sb("u", [128, 2, 512], F32)}

    def normA_phase(l, hT):
        with kb.scope():
            tmp = norm_tmp()
            xts = [kb.sb("xt", [128, 8, 512], F32) for _ in range(2)]
            xin = [kb.sb("xin", [128, D], F32) for _ in range(3)]
            nin = 0
            st_ = {"nin": 0}

            def T_(ti):
                t0, w, isc = TILES[ti]
                xt = xts[ti % 2]
                if l == 0:
                    for c4 in range(w // 128):
                        xi = xin[st_["nin"] % 3]
                        st_["nin"] += 1
                        src = ctx_d.t[c4 * 128:(c4 + 1) * 128, :] if isc else x_d.t[t0 + c4 * 128:t0 + (c4 + 1) * 128, :]
                        kb.dma("sp", xi[:], src, writes=[xi.r()])
                        for kh in range(2):
                            pb = PB[(c4 * 2 + kh) % 4]
                            for kk in range(4):
                                k = kh * 4 + kk
                                kb.op("pe", lambda e: e.transpose(pb[:, kk * 128:(kk + 1) * 128], xi[:, k * 128:(k + 1) * 128], ident[:]),
                                      [xi.r(), ident.r()], [pb.r()])
                            cp("act" if kh == 0 else "dve", xt[:, kh * 4:(kh + 1) * 4, c4 * 128:(c4 + 1) * 128],
                               pb[:, :].rearrange("p (a b) -> p a b", b=128), [pb.r()], [xt.r()])
                    kb.dma("sp", xTv[:, :, t0:t0 + w], xt[:, :, :w], reads=[xt.r()], writes=[xT_d.r(ti)])
                else:
                    kb.dma("sp", xt[:, :, :w], xTv[:, :, t0:t0 + w], reads=[xT_d.r(ti)], writes=[xt.r()])

            T_(0)
            for ti, (t0, w, isc) in enumerate(TILES):
                if ti + 1 < len(TILES):
                    T_(ti + 1)
                norm_tile(l, 0, xts[ti % 2], ti, lambda k, t0=t0, w=w, ti=ti: (hT[:, k, t0:t0 + w], [hT.r(ti)]), tmp)

    def attn_groups(l, hT, ycatT):
        src_w = win_d.t[l].rearrange("(k p) n -> p k n", p=128)
        with kb.scope():
            qT = kb.sb("qT", [128, NTOK], BF16)
            kT = kb.sb("kT", [128, NTOK], BF16)
            vtm = kb.sb("vtm", [128, 34, 128], BF16)
            wgs = [kb.sb("wg", [128, 8, 384], BF16) for _ in range(2)]
            E = [kb.sb("E", [128, 512], BF16) for _ in range(4)]
            GA0 = 3 * (DHY + 384)
            NA0 = 3 * DHY
            PCOLS = [(GA0 + gp * 128, GA0 + 256 + gp * 128, GA0 + 512 + gp * 128) for gp in range(2)] + \
                    [(NA0 + np_ * 128, NA0 + 384 + np_ * 128, NA0 + 768 + np_ * 128) for np_ in range(3)]
            pstate = {"n": 0}

            def load_wg(idx):
                if idx >= len(PCOLS):
                    return
                for j, c0 in enumerate(PCOLS[idx]):
                    kb.dma("pool", wgs[idx % 2][:, :, j * 128:(j + 1) * 128], src_w[:, :, c0:c0 + 128], writes=[wgs[idx % 2].r(j)])

            load_wg(0)
            tmp = {"sq2": [kb.sb("sqa", [128, 512], BF16) for _ in range(2)], "rt": kb.sb("rta", [128, 512], F32), "R": kb.sb("Ra", [128, 512], F32),
                   "Rd": [kb.sb("Rd", [128, 512], F32) for _ in range(1)]}
            state = {"ob": 0, "cs": 0}

            def project(colq, colk, colv, gq, gk, rope):
                pidx = pstate["n"]
                pstate["n"] += 1
                assert PCOLS[pidx] == (colq, colk, colv)
                wg = wgs[pidx % 2]
                dst = ((qT, gq), (kT, gk))
                nT = len(TILES)

                def A(ti):
                    t0, w, isc = TILES[ti]
                    for j in range(2):
                        ps = PB[(ti % 3) * 2 + j]
                        for k in range(KC):
                            mm(ps[:, :w], wg[:, k, j * 128:(j + 1) * 128], hT[:, k, t0:t0 + w], k == 0, k == KC - 1,
                               [wg.r(j), hT.r(ti)], [ps.r()])

                def S_(ti):
                    t0, w, isc = TILES[ti]
                    for j in range(2):
                        ps = PB[(ti % 3) * 2 + j]
                        sq = tmp["sq2"][j]
                        pb = PB[6 + j]
                        act(sq[:, :w], ps[:, :w], AF.Square, [ps.r()], [sq.r()])
                        mm(pb[:, :w], blk_b[:], sq[:, :w], True, True, [blk_b.r(), sq.r()], [pb.r()])

                def F_(ti):
                    t0, w, isc = TILES[ti]
                    for j in range(2):
                        ps = PB[(ti % 3) * 2 + j]
                        pb = PB[6 + j]
                        dstt, gcol = dst[j]
                        out_ap, out_res = dstt[:, t0:t0 + w], [dstt.r(ti)]
                        act(tmp["rt"][:, :w], pb[:, :w], AF.Ln, [pb.r()], [tmp["rt"].r()], scale=1.0 / 64, bias=EPS)
                        act(tmp["R"][:, :w], tmp["rt"][:, :w], AF.Exp, [tmp["rt"].r()], [tmp["R"].r()], scale=-0.5)
                        if not (rope and not isc):
                            stt(out_ap, ps[:, :w], gcol, tmp["R"][:, :w], ALU.mult, ALU.mult, [ps.r(), prm.r(), tmp["R"].r()], out_res)
                            continue
                        qn = tmp["qn2"][j]
                        stt(qn[:, :w], ps[:, :w], gcol, tmp["R"][:, :w], ALU.mult, ALU.mult, [ps.r(), prm.r(), tmp["R"].r()], [qn.r()])
                        cs = tmp["cs"][state["cs"] % 2]
                        state["cs"] += 1
                        kb.dma("sp", cs[:, 0, :w], cd["cosT"][:, t0:t0 + w], writes=[cs.r()])
                        kb.dma("sp", cs[:, 1, :w], cd["sinT"][:, t0:t0 + w], writes=[cs.r()])
                        mm(pb[:, :w], rm_b[:], qn[:, :w], True, True, [rm_b.r(), qn.r()], [pb.r()])
                        tt("pool", tmp["t1"][:, :w], qn[:, :w], cs[:, 0, :w], ALU.mult, [qn.r(), cs.r()], [tmp["t1"].r()])
                        tt("dve", tmp["t2"][:, :w], pb[:, :w], cs[:, 1, :w], ALU.mult, [pb.r(), cs.r()], [tmp["t2"].r()])
                        tt("pool", out_ap, tmp["t1"][:, :w], tmp["t2"][:, :w], ALU.add, [tmp["t1"].r(), tmp["t2"].r()], out_res)

                A(0)
                A(1)
                for ti in range(nT):
                    S_(ti)
                    if ti + 2 < nT:
                        A(ti + 2)
                    F_(ti)
                for c in range(34):
                    ps = PB[4 + c % 2]
                    ti = min(c // 4, 8)
                    for k in range(KC):
                        mm(ps[:, 0:128], hT[:, k, c * 128:(c + 1) * 128], wg[:, k, 256:384], k == 0, k == KC - 1,
                           [hT.r(ti), wg.r(2)], [ps.r()])
                    cp("act" if c % 2 == 0 else "dve", vtm[:, c, :], ps[:, 0:128], [ps.r()], [vtm.r(c)])
                load_wg(pidx + 1)

            STB = [PB[0], PB[1], PB[2], PB[7]]

            def dense_attn2(q0, w, kchunks, out_ap, out_res):
                ob = state["ob"] % 2
                state["ob"] += 1
                o1, o2 = PB[3 + 2 * ob], PB[4 + 2 * ob]
                n = len(kchunks)

                def qk(i):
                    kc = kchunks[i]
                    for hl in range(2):
                        lo, hi = 64 * hl, 64 * hl + 64
                        st = STB[(2 * i + hl) % 4]
                        mm(st[:, :w], kT[lo:hi, kc * 128:(kc + 1) * 128], qT[lo:hi, q0:q0 + w], True, True,
                           [kT.r(min(kc // 4, 8)), qT.r(min(q0 // 512, 8))], [st.r()])

                def rest(i):
                    kc = kchunks[i]
                    for hl in range(2):
                        st = STB[(2 * i + hl) % 4]
                        e_ = E[(2 * i + hl) % 4]
                        act(e_[:, :w], st[:, :w], AF.Exp, [st.r()], [e_.r()])
                    for hl in range(2):
                        lo, hi = 64 * hl, 64 * hl + 64
                        e_ = E[(2 * i + hl) % 4]
                        mm(o1[lo:hi, :w], vtm[:, kc, lo:hi], e_[:, :w], i == 0, i == n - 1, [vtm.r(kc), e_.r()], [o1.r()])
                    for hl in range(2):
                        lo, hi = 64 * hl, 64 * hl + 64
                        e_ = E[(2 * i + hl) % 4]
                        mm(o2[lo:hi, :w], ones_b[:, 0:64], e_[:, :w], i == 0, i == n - 1, [ones_b.r(), e_.r()], [o2.r()])

                qk(0)
                for i in range(n):
                    if i + 1 < n:
                        qk(i + 1)
                    rest(i)
                rd = tmp["Rd"][0]
                act(rd[:, :w], o2[:, :w], AF.Ln, [o2.r()], [rd.r()])
                act(rd[:, :w], rd[:, :w], AF.Exp, [rd.r()], [rd.r()], scale=-1.0)
                tt("dve", out_ap, o1[:, :w], rd[:, :w], ALU.mult, [o1.r(), rd.r()], out_res)

            def na_attn2(chunk, Mts):
                cur = {}

                def geom(r):
                    w0 = min(max(r - 4, 0), 56)
                    par = w0 % 2
                    c0 = w0 // 2
                    nch = 4 + par
                    vi = NA_VARIANTS.index((par, w0 - r))
                    return nch, vi, [c0 + ch for ch in range(nch)] + [32, 33]

                def qk(r):
                    nch, vi, chunks = geom(r)
                    for ci, kc in enumerate(chunks):
                        for hl in range(2):
                            lo, hi = 64 * hl, 64 * hl + 64
                            st = STB[(2 * r + hl) % 4]
                            mm(st[:, ci * 64:(ci + 1) * 64], kT[lo:hi, kc * 128:(kc + 1) * 128], qT[lo:hi, r * 64:(r + 1) * 64], True, True,
                               [kT.r(min(kc // 4, 8)), qT.r(r // 8)], [st.r()])

                def rest(r):
                    r8, rr = divmod(r, 8)
                    if rr == 0:
                        cur["ob"] = state["ob"] % 2
                        state["ob"] += 1
                    ob = cur["ob"]
                    o1, o2 = PB[3 + 2 * ob], PB[4 + 2 * ob]
                    nch, vi, chunks = geom(r)
                    nk = len(chunks)
                    for hl in range(2):
                        st = STB[(2 * r + hl) % 4]
                        e_ = E[(2 * r + hl) % 4]
                        act(e_[:, :nk * 64], st[:, :nk * 64], AF.Exp, [st.r()], [e_.r()])
                        tt("dve", e_[:, :nch * 64], e_[:, :nch * 64], Mts[hl][:, vi, 0:nch, :].rearrange("p a b -> p (a b)"), ALU.mult,
                           [e_.r(), Mts[hl].r()], [e_.r()])
                    for ci, kc in enumerate(chunks):
                        for hl in range(2):
                            lo, hi = 64 * hl, 64 * hl + 64
                            e_ = E[(2 * r + hl) % 4]
                            mm(o1[lo:hi, rr * 64:(rr + 1) * 64], vtm[:, kc, lo:hi], e_[:, ci * 64:(ci + 1) * 64], ci == 0, ci == nk - 1,
                               [vtm.r(kc), e_.r()], [o1.r()])
                    for ci, kc in enumerate(chunks):
                        for hl in range(2):
                            lo, hi = 64 * hl, 64 * hl + 64
                            e_ = E[(2 * r + hl) % 4]
                            mm(o2[lo:hi, rr * 64:(rr + 1) * 64], ones_b[:, 0:64], e_[:, ci * 64:(ci + 1) * 64], ci == 0, ci == nk - 1,
                               [ones_b.r(), e_.r()], [o2.r()])
                    if rr == 7:
                        rd = tmp["Rd"][0]
                        act(rd[:, :], o2[:, :], AF.Ln, [o2.r()], [rd.r()])
                        act(rd[:, :], rd[:, :], AF.Exp, [rd.r()], [rd.r()], scale=-1.0)
                        tt("dve", ycatT[:, chunk, r8 * 512:(r8 + 1) * 512], o1[:, :], rd[:, :], ALU.mult, [o1.r(), rd.r()],
                           [ycatT.r((chunk, r8))])

                qk(0)
                for r in range(64):
                    if r + 1 < 64:
                        qk(r + 1)
                    rest(r)

            with kb.scope():
                tmp["qn2"] = [kb.sb("qn", [128, 512], BF16) for _ in range(2)]
                tmp["t1"] = kb.sb("t1", [128, 512], F32)
                tmp["t2"] = kb.sb("t2", [128, 512], F32)
                tmp["cs"] = [kb.sb("cs", [128, 2, 512], BF16) for _ in range(2)]
                for gp in range(2):
                    base = 3 * (DHY + 384)
                    mark("gaP%d" % gp)
                    project(base + gp * 128, base + 256 + gp * 128, base + 512 + gp * 128, prm[:, l, 65:66], prm[:, l, 55:56], True)
                    if stop == "proj":
                        dbg_dump("qT", qT[:], [128, NTOK], BF16)
                        dbg_dump("kT", kT[:], [128, NTOK], BF16)
                        dbg_dump("vtm", vtm[:], [128, 34, 128], BF16)
                        return
                    mark("gaA%d" % gp)
                    for qt in range(8):
                        dense_attn2(qt * 512, 512, list(range(34)), ycatT[:, 6 + gp, qt * 512:(qt + 1) * 512], [ycatT.r((6 + gp, qt))])
                    if l == 0:
                        dense_attn2(S, LC, [32, 33], ycatT[:, 6 + gp, S:NTOK], [ycatT.r((6 + gp, 8))])
            with kb.scope():
                ec2 = kb.sb("ec2", [128, 15, 64], BF16)
                Mts = [kb.sb("Mt", [128, len(NA_VARIANTS), 5, 64], BF16) for _ in range(2)]
                cmask = kb.sb("cmask", [128, 64], F32)
                stage = kb.sb("stage", [128, 15, 64], F32)
                kb.dma("sp", cmask[:], cd["colmask"][:, :], writes=[cmask.r()])
                for np_ in range(3):
                    base = 3 * DHY
                    mark("naP%d" % np_)
                    project(base + np_ * 128, base + 384 + np_ * 128, base + 768 + np_ * 128, prm[:, l, 64:65], prm[:, l, 53:54], False)
                    mark("naA%d" % np_)
                    for hl in range(2):
                        h = np_ * 2 + hl
                        Mt = Mts[hl]
                        for half in range(2):
                            kb.dma("sp", stage[half * 64:(half + 1) * 64], rpb_d.t[l, :, h * 15:(h + 1) * 15, :], writes=[stage.r()])
                        act(stage[:], stage[:], AF.Exp, [stage.r()], [stage.r()])
                        tt("dve", ec2[:], stage[:], cmask[:].unsqueeze(1).to_broadcast([128, 15, 64]), ALU.mult, [stage.r(), cmask.r()], [ec2.r()])
                        kb.op("pool", lambda e: e.memset(Mt[:], 0.0), [], [Mt.r()])
                        for vi, (par, dwr) in enumerate(NA_VARIANTS):
                            for krl in range(2):
                                chs = [ch for ch in range(5) if 0 <= 2 * ch + krl - par < 8]
                                lo_ch, n = chs[0], len(chs)
                                dr0 = 2 * lo_ch + krl - par + dwr + 7
                                cp("pool", Mt[64 * krl:64 * krl + 64, vi, lo_ch:lo_ch + n, :], ec2[64 * krl:64 * krl + 64, dr0:dr0 + 2 * n - 1:2, :],
                                   [ec2.r()], [Mt.r()])
                    na_attn2(3 + np_, Mts)
                    if l == 0:
                        dense_attn2(S, LC, [32, 33], ycatT[:, 3 + np_, S:NTOK], [ycatT.r((3 + np_, 8))])

    def hyena_proj(l, hT, ztm, ztc):
        src_w = win_d.t[l].rearrange("(k p) n -> p k n", p=128)
        RW = NTOK + 4
        with kb.scope():
            wg = kb.sb("wgh", [128, 8, 384], BF16)
            raw = [kb.sb("raw", [128, RW], BF16) for _ in range(2)]
            u1 = kb.sb("u1", [128, NTOK], BF16)
            zf = kb.sb("zf", [128, NTOK], BF16)
            x0 = kb.sb("x0", [128, NTOK], BF16)
            tc_ = [kb.sb("tc", [128, 1024], F32) for _ in range(2)]
            for rw in raw:
                for c in (0, S + 1, S + 2, RW - 1):
                    kb.op("pool", lambda e: e.memset(rw[:, c:c + 1], 0.0), [], [rw.r()])
            nraw = [0]
            pbf = PB[7].t[:, :].bitcast(BF16)

            def conv_chunk(cidx, j, mul_by=None, out=None, out_res=None):
                rw = raw[nraw[0] % 2]
                nraw[0] += 1
                for ti, (t0, w, isc) in enumerate(TILES):
                    ps = PB[ti % 4]
                    for k in range(KC):
                        mm(ps[:, :w], wg[:, k, j * 128:(j + 1) * 128], hT[:, k, t0:t0 + w], k == 0, k == KC - 1, [wg.r(j), hT.r(ti)], [ps.r()])
                    off = (S + 3) if isc else (1 + t0)
                    cp("act", rw[:, off:off + w], ps[:, :w], [ps.r()], [rw.r()])
                pc = 16 + cidx * 4
                segs = [(1 + a, a, 1024) for a in range(0, S, 1024)] + [(S + 3, S, LC)]
                for si, (ro, oo, n) in enumerate(segs):
                    t = tc_[si % 2]
                    ts("dve", t[:, :n], rw[:, ro:ro + n], prm[:, l, pc + 1:pc + 2], prm[:, l, pc + 3:pc + 4], ALU.mult, ALU.add, [rw.r(), prm.r()], [t.r()])
                    stt(t[:, :n], rw[:, ro - 1:ro - 1 + n], prm[:, l, pc:pc + 1], t[:, :n], ALU.mult, ALU.add, [rw.r(), prm.r(), t.r()], [t.r()])
                    if mul_by is None:
                        stt(out[:, oo:oo + n], rw[:, ro + 1:ro + 1 + n], prm[:, l, pc + 2:pc + 3], t[:, :n], ALU.mult, ALU.add, [rw.r(), prm.r(), t.r()], out_res)
                    else:
                        stt(t[:, :n], rw[:, ro + 1:ro + 1 + n], prm[:, l, pc + 2:pc + 3], t[:, :n], ALU.mult, ALU.add, [rw.r(), prm.r(), t.r()], [t.r()])
                        tt("pool", out[:, oo:oo + n], t[:, :n], mul_by[:, oo:oo + n], ALU.mult, [t.r(), mul_by.r()], out_res)

            for j in range(3):
                for jj, cidx in enumerate((j, 3 + j, 6 + j)):
                    kb.dma("pool", wg[:, :, jj * 128:(jj + 1) * 128], src_w[:, :, cidx * 128:(cidx + 1) * 128], writes=[wg.r(jj)])
                conv_chunk(3 + j, 1, out=u1, out_res=[u1.r()])
                conv_chunk(6 + j, 2, mul_by=u1, out=zf, out_res=[zf.r()])
                conv_chunk(j, 0, out=x0, out_res=[x0.r()])
                kb.dma("sp", x0c_d.t[:, j, :], x0[:], reads=[x0.r()], writes=[x0c_d.r()])
                for g in range(5):
                    nt = 8 if g < 4 else 2
                    for i in range(nt):
                        tcn = g * 8 + i
                        kb.op("pe", lambda e: e.transpose(pbf[:, i * 128:(i + 1) * 128], zf[:, tcn * 128:(tcn + 1) * 128], ident_b[:]),
                              [zf.r(), ident_b.r()], [PB[7].r()])
                    if g < 4:
                        cp("act", ztm[:, g * 8:(g + 1) * 8, j * 128:(j + 1) * 128], pbf[:, :].rearrange("p (a b) -> p a b", b=128), [PB[7].r()], [ztm.r()])
                    else:
                        cp("act", ztc[:, :, j * 128:(j + 1) * 128], pbf[:, 0:256].rearrange("p (a b) -> p a b", b=128), [PB[7].r()], [ztc.r()])

    def hyena_filter(l, L, hsum, hdiff):
        NT = L // 128
        W = min(512, L)
        with kb.scope():
            h3 = kb.sb("h3", [64, L], F32)
            zp = [kb.sb("zp", [33, 512], F32) for _ in range(2)]
            w1 = kb.sb("w1", [33, 64], F32)
            w2 = kb.sb("w2", [64, 2, 64], F32)
            w3 = kb.sb("w3", [64, 768], F32)
            hv = kb.sb("hv", [64, 8], F32)
            negt = kb.sb("negt", [128, NT], F32)
            dabs = kb.sb("dabs", [128, 384], F32)
            skip = kb.sb("skip", [1, 384], F32)
            arg = [kb.sb("arg", [64, 512], F32) for _ in range(2)]
            mk = [kb.sb("mk", [64, 512], F32) for _ in range(2)]
            hh = [kb.sb("hh", [64, 512], F32) for _ in range(2)]
            dec = [kb.sb("dec", [128, 384], F32) for _ in range(2)]
            hf = [kb.sb("hf", [128, 384], F32) for _ in range(2)]
            hb = [kb.sb("hb", [128, 384], F32) for _ in range(2)]
            kb.dma("sp", w1[:], hyw1_d.t[l], writes=[w1.r()])
            kb.dma("sp", w2[:, 0, :], hyw2_d.t[l, 0], writes=[w2.r()])
            kb.dma("sp", w2[:, 1, :], hyw2_d.t[l, 1], writes=[w2.r()])
            kb.dma("sp", w3[:], hyw3_d.t[l], writes=[w3.r()])
            kb.dma("sp", hv[:, 0:4], hyv_d.t[l], writes=[hv.r()])
            kb.dma("sp", negt[:], cd["negt%d" % L][:, :], writes=[negt.r()])
            kb.dma("sp", dabs[:], cd["dabs"][:, :], writes=[dabs.r()])
            kb.dma("sp", skip[:], hysk_d.t[l], writes=[skip.r()])
            ts("dve", hv[:, 4:7], hv[:, 1:4], hv[:, 0:1], None, ALU.mult, None, [hv.r()], [hv.r()])
            cnt = [0]

            def sinact(ps, w, fbcol, out_ap, out_res):
                i = cnt[0] % 2
                cnt[0] += 1
                a, m = arg[i], mk[i]
                ts("dve", a[:, :w], ps[0:64, :w], hv[:, 0:1], hv[:, fbcol:fbcol + 1], ALU.mult, ALU.add, [ps.r(), hv.r()], [a.r()])
                ts("dve", m[:, :w], a[:, :w], PI, -2.0 * PI, ALU.is_gt, ALU.mult, [a.r()], [m.r()])
                tt("pool", a[:, :w], a[:, :w], m[:, :w], ALU.add, [a.r(), m.r()], [a.r()])
                ts("dve", m[:, :w], a[:, :w], -PI, 2.0 * PI, ALU.is_lt, ALU.mult, [a.r()], [m.r()])
                tt("pool", a[:, :w], a[:, :w], m[:, :w], ALU.add, [a.r(), m.r()], [a.r()])
                act(out_ap, a[:, :w], AF.Sin, [a.r()], out_res)

            for tix in range(L // W):
                z = zp[tix % 2]
                kb.dma("sp", z[:, :W], cd["zpos%d" % L][:, tix * W:(tix + 1) * W], writes=[z.r()])
                ps = PB[tix % 2]
                mm(ps[0:64, :W], w1[:], z[:, :W], True, True, [w1.r(), z.r()], [ps.r()])
                h_ = hh[0]
                sinact(ps, W, 4, h_[:, :W], [h_.r()])
                ps2 = PB[2 + tix % 2]
                mm(ps2[0:64, :W], w2[:, 0, :], h_[:, :W], True, True, [w2.r(), h_.r()], [ps2.r()])
                h2_ = hh[1]
                sinact(ps2, W, 5, h2_[:, :W], [h2_.r()])
                ps3 = PB[4 + tix % 2]
                mm(ps3[0:64, :W], w2[:, 1, :], h2_[:, :W], True, True, [w2.r(), h2_.r()], [ps3.r()])
                sinact(ps3, W, 6, h3[:, tix * W:(tix + 1) * W], [h3.r()])
            for tc in range(NT):
                i = tc % 2
                pf, pb_ = PB[i * 2], PB[i * 2 + 1]
                mm(pf[:, 0:384], h3[:, tc * 128:(tc + 1) * 128], w3[:, 0:384], True, True, [h3.r(), w3.r()], [pf.r()])
                mm(pb_[:, 0:384], h3[:, tc * 128:(tc + 1) * 128], w3[:, 384:768], True, True, [h3.r(), w3.r()], [pb_.r()])
                act(dec[i][:], dabs[:], AF.Exp, [dabs.r(), negt.r()], [dec[i].r()], scale=negt[:, tc:tc + 1])
                tt("dve", hf[i][:], pf[:, 0:384], dec[i][:], ALU.mult, [pf.r(), dec[i].r()], [hf[i].r()])
                tt("dve", hb[i][:], pb_[:, 0:384], dec[i][:], ALU.mult, [pb_.r(), dec[i].r()], [hb[i].r()])
                if tc == 0:
                    kb.op("dve", lambda e: e.memset(hb[i][0:1, :], 0.0), [], [hb[i].r()])
                    tt("dve", hf[i][0:1, :], hf[i][0:1, :], skip[:], ALU.add, [hf[i].r(), skip.r()], [hf[i].r()])
                tt("pool", hsum[:, tc, :], hf[i][:], hb[i][:], ALU.add, [hf[i].r(), hb[i].r()], [hsum.r()])
                tt("pool", hdiff[:, tc, :], hb[i][:], hf[i][:], ALU.subtract, [hf[i].r(), hb[i].r()], [hdiff.r()])
            dbg_dump("hsum%d_%d" % (l, L), hsum[:], [128, NT, 384], BF16)

    def hyena_dft(l, L, ztm, ycatT, col0):
        NT = L // 128
        W = min(512, L)
        NP = max(1, NT // 8)
        FP = NT // NP
        NH = 2 if NT >= 16 else 1
        HT = NT // NH
        with kb.scope():
            hsum = kb.sb("hsum", [128, NT, 384], BF16)
            hdiff = kb.sb("hdiff", [128, NT, 384], BF16)
            hyena_filter(l, L, hsum, hdiff)
            pq = kb.sb("pq", [128, NT, 2, 384], BF16)
            with kb.scope():
                cbuf = [kb.sb("cbuf", [128, HT, 128], BF16) for _ in range(3)]
                sbuf = [kb.sb("sbuf", [128, HT, 128], BF16) for _ in range(3)]
                t1 = [kb.sb("t1h", [128, 384], F32) for _ in range(2)]
                t2 = [kb.sb("t2h", [128, 384], F32) for _ in range(2)]
                nb = 0
                for ps_ in range(2):
                    mc, ms = (hsum, hdiff) if ps_ == 0 else (ztm, ztm)
                    for fc in range(NT):
                        ba, bb = PB[(fc % 2) * 2], PB[(fc % 2) * 2 + 1]
                        for hh_ in range(NH):
                            cb, sb_ = cbuf[nb % 3], sbuf[nb % 3]
                            nb += 1
                            kb.dma("sp", cb[:], cd["FWC%d" % L][fc, :, hh_ * HT:(hh_ + 1) * HT, :], writes=[cb.r()])
                            kb.dma("sp", sb_[:], cd["FWS%d" % L][fc, :, hh_ * HT:(hh_ + 1) * HT, :], writes=[sb_.r()])
                            for t_ in range(HT):
                                tc = hh_ * HT + t_
                                mm(ba[:, 0:384], cb[:, t_, :], mc[:, tc, :], tc == 0, tc == NT - 1, [cb.r(), mc.r()], [ba.r()])
                                mm(bb[:, 0:384], sb_[:, t_, :], ms[:, tc, :], tc == 0, tc == NT - 1, [sb_.r(), ms.r()], [bb.r()])
                        if ps_ == 0:
                            cp("act", pq[:, fc, 0, :], ba[:, 0:384], [ba.r()], [pq.r(fc)])
                            cp("dve", pq[:, fc, 1, :], bb[:, 0:384], [bb.r()], [pq.r(fc)])
                        else:
                            i = fc % 2
                            tt("dve", t1[i][:], ba[:, 0:384], pq[:, fc, 0, :], ALU.mult, [ba.r(), pq.r(fc)], [t1[i].r()])
                            tt("dve", t2[i][:], bb[:, 0:384], pq[:, fc, 1, :], ALU.mult, [bb.r(), pq.r(fc)], [t2[i].r()])
                            tt("pool", t1[i][:], t1[i][:], t2[i][:], ALU.add, [t1[i].r(), t2[i].r()], [t1[i].r()])
                            tt("dve", t2[i][:], bb[:, 0:384], pq[:, fc, 0, :], ALU.mult, [bb.r(), pq.r(fc)], [t2[i].r()])
                            cp("pool", pq[:, fc, 0, :], t1[i][:], [t1[i].r()], [pq.r(fc)])
                            tt("dve", t1[i][:], ba[:, 0:384], pq[:, fc, 1, :], ALU.mult, [ba.r(), pq.r(fc)], [t1[i].r()])
                            tt("pool", pq[:, fc, 1, :], t2[i][:], t1[i][:], ALU.subtract, [t1[i].r(), t2[i].r()], [pq.r(fc)])
                    kb.barrier()
            with kb.scope():
                ivc = [kb.sb("ivc", [128, FP, W], BF16) for _ in range(2)]
                ivs = [kb.sb("ivs", [128, FP, W], BF16) for _ in range(2)]
                x0t = [kb.sb("x0t", [128, 3, W], BF16) for _ in range(2)]
                nb = 0
                for tti in range(L // W):
                    accs = [PB[(tti % 2) * 3 + cc] for cc in range(3)]
                    xt_ = x0t[tti % 2]
                    c0 = col0 + tti * W
                    kb.dma("sp", xt_[:], x0c_d.t[:, :, c0:c0 + W], reads=[x0c_d.r()], writes=[xt_.r()])
                    for part in range(NP):
                        ic, is_ = ivc[nb % 2], ivs[nb % 2]
                        nb += 1
                        kb.dma("sp", ic[:], cd["IVC%d" % L][tti, :, part * FP:(part + 1) * FP, :], writes=[ic.r()])
                        kb.dma("sp", is_[:], cd["IVS%d" % L][tti, :, part * FP:(part + 1) * FP, :], writes=[is_.r()])
                        for fi in range(FP):
                            fc = part * FP + fi
                            for cc in range(3):
                                first = (part == 0 and fi == 0)
                                last = (part == NP - 1 and fi == FP - 1)
                                mm(accs[cc][:, :W], pq[:, fc, 0, cc * 128:(cc + 1) * 128], ic[:, fi, :], first, False, [pq.r(fc), ic.r()], [accs[cc].r()])
                                mm(accs[cc][:, :W], pq[:, fc, 1, cc * 128:(cc + 1) * 128], is_[:, fi, :], False, last, [pq.r(fc), is_.r()], [accs[cc].r()])
                    for cc in range(3):
                        stt(ycatT[:, cc, c0:c0 + W], accs[cc][:, :W], 1.0 / L, xt_[:, cc, :], ALU.mult, ALU.mult, [accs[cc].r(), xt_.r()],
                            [ycatT.r((cc, c0 // 512))])

    def group_norm(l, ycatT, ntiles):
        with kb.scope():
            tmp = {"sq": kb.sb("sqg", [128, 2, 512], BF16), "rt": kb.sb("rtg", [128, 512], F32), "R": kb.sb("Rg", [128, 512], F32)}
            for ti in range(ntiles):
                t0, w, isc = TILES[ti]
                for gi, (ks, n) in enumerate((((0, 1, 2), 384), ((3, 4, 5), 384), ((6, 7), 256))):
                    pb = PB[(ti * 3 + gi) % 4]
                    rms_tile(w, n, [(ycatT[:, k, t0:t0 + w], [ycatT.r((k, ti))]) for k in ks], pb, tmp)
                    for k in ks:
                        stt(ycatT[:, k, t0:t0 + w], ycatT[:, k, t0:t0 + w], prm[:, l, 56 + k:57 + k], tmp["R"][:, :w], ALU.mult, ALU.mult,
                            [ycatT.r((k, ti)), prm.r(), tmp["R"].r()], [ycatT.r((k, ti))])

    def router_tile(ti, h32):
        maskall, comball = rt["mask"], rt["comb"]
        for c4 in range(4):
            ch = ti * 4 + c4
            ps = PB[4]
            for k in range(KC):
                mm(ps[:, 0:8], h32[:, k, c4 * 128:(c4 + 1) * 128], wr32[:, k, :], k == 0, k == KC - 1, [h32.r(), wr32.r()], [ps.r()])
            cp("dve", lg[:, 0:8], ps[:, 0:8], [ps.r()], [lg.r()])
            kb.op("dve", lambda e: e.max(out=lg[:, 8:16], in_=lg[:, 0:8]), [lg.r()], [lg.r()])
            ts("dve", maskall[:, ch, :], lg[:, 0:8], lg[:, 9:10], None, ALU.is_ge, None, [lg.r()], [maskall.r()])
            ts("dve", lg[:, 24:25], lg[:, 8:9], -1.0, None, ALU.mult, None, [lg.r()], [lg.r()])
            act(lg[:, 32:40], lg[:, 0:8], AF.Exp, [lg.r()], [lg.r()], bias=lg[:, 24:25])
            tt("dve", lg[:, 32:40], lg[:, 32:40], maskall[:, ch, :], ALU.mult, [lg.r(), maskall.r()], [lg.r()])
            kb.op("dve", lambda e: e.tensor_reduce(out=lg[:, 25:26], in_=lg[:, 32:40], axis=AX.X, op=ALU.add), [lg.r()], [lg.r()])
            recip(lg[:, 26:27], lg[:, 25:26], [lg.r()], [lg.r()])
            ts("dve", comball[:, ch, :], lg[:, 32:40], lg[:, 26:27], None, ALU.mult, None, [lg.r()], [comball.r()])

    def wout_phase(l, ycatT, ntiles, moe):
        with kb.scope():
            wo = kb.sb("wo", [128, 8, D], BF16)
            kb.dma("pool", wo[:], wout_d.t[l].rearrange("(k p) n -> p k n", p=128), writes=[wo.r()])
            tmp = norm_tmp()
            xo = [kb.sb("xo", [128, 8, 512], F32) for _ in range(2)]
            xn = [kb.sb("xn", [128, 8, 512], F32) for _ in range(2)]
            h2t = [kb.sb("h2t", [128, 8, 512], BF16) for _ in range(1 if moe else 2)]
            h32 = kb.sb("h32", [128, 8, 512], F32) if moe else None
            htm = [kb.sb("htm", [128, D], BF16) for _ in range(2)] if moe else None
            xtm = [kb.sb("xtm", [128, D], F32) for _ in range(2)] if moe else None
            pbf6 = PB[6].t[:, :].bitcast(BF16)
            if moe:
                rt["mask"] = kb.sb("maskall", [128, 32, 8], BF16)
                rt["comb"] = kb.sb("comball", [128, 32, 8], F32)
            def W_(ti):
                t0, w, isc = TILES[ti]
                wh = 1 if isc else 0
                xo_, xn_ = xo[ti % 2], xn[ti % 2]
                if ti == 0:
                    kb.dma("sp", xo_[:, :, :w], xTv[:, :, t0:t0 + w], reads=[xT_d.r(ti)], writes=[xo_.r()])
                if ti + 1 < ntiles:
                    t1_, w1_, _ = TILES[ti + 1]
                    kb.dma("sp", xo[(ti + 1) % 2][:, :, :w1_], xTv[:, :, t1_:t1_ + w1_], reads=[xT_d.r(ti + 1)], writes=[xo[(ti + 1) % 2].r()])
                for oc in range(KC):
                    ps = PB[oc % 4]
                    for k in range(KC):
                        mm(ps[:, :w], wo[:, k, oc * 128:(oc + 1) * 128], ycatT[:, k, t0:t0 + w], k == 0, k == KC - 1,
                           [wo.r(), ycatT.r((k, ti))], [ps.r()])
                    stt(xn_[:, oc, :w], ps[:, :w], aff[:, 2, oc, wh:wh + 1], xo_[:, oc, :w], ALU.mult, ALU.add, [ps.r(), aff.r(), xo_.r()], [xn_.r()])
                if not moe:
                    kb.dma("sp", xTv[:, :, t0:t0 + w], xn_[:, :, :w], reads=[xn_.r()], writes=[xT_d.r(ti)])

            def N_(ti):
                t0, w, isc = TILES[ti]
                xn_, h2_ = xn[ti % 2], h2t[ti % len(h2t)]
                norm_tile(l, 1, xn_, ti, lambda k: (h2_[:, k, :w], [h2_.r()]), tmp, h32=h32)
                if not moe:
                    kb.dma("sp", h2_d.t[:, :, t0:t0 + w], h2_[:, :, :w], reads=[h2_.r()], writes=[h2_d.r(ti)])
                    return
                router_tile(ti, h32)
                for c4 in range(4):
                    r0 = t0 + c4 * 128
                    hb, xb = htm[c4 % 2], xtm[c4 % 2]
                    for k in range(KC):
                        kb.op("pe", lambda e: e.transpose(pbf6[:, k * 128:(k + 1) * 128], h2_[:, k, c4 * 128:(c4 + 1) * 128], ident_b[:]),
                              [h2_.r(), ident_b.r()], [PB[6].r()])
                    cp("act", hb[:], pbf6[:, :], [PB[6].r()], [hb.r()])
                    kb.dma("sp", h2tm_d.t[r0:r0 + 128, :], hb[:], reads=[hb.r()], writes=[h2tm_d.r(ti * 4 + c4)])
                    for kh in range(2):
                        pb = PB[5]
                        for kk in range(4):
                            k = kh * 4 + kk
                            kb.op("pe", lambda e: e.transpose(pb[:, kk * 128:(kk + 1) * 128], xn_[:, k, c4 * 128:(c4 + 1) * 128], ident[:]),
                                  [xn_.r(), ident.r()], [pb.r()])
                        cp("dve" if kh == 0 else "act", xb[:, kh * 512:(kh + 1) * 512], pb[:, :], [pb.r()], [xb.r()])
                    kb.dma("sp", xtm_d.t[r0:r0 + 128, :], xb[:], reads=[xb.r()], writes=[xtm_d.r(ti * 4 + c4)])

            W_(0)
            for ti in range(ntiles):
                if ti + 1 < ntiles:
                    W_(ti + 1)
                N_(ti)
            if moe:
                kb.dma("sp", mask_d.t[:, :], rt["mask"][:].rearrange("p c e -> p (c e)"), reads=[rt["mask"].r()], writes=[mask_d.r()])
                kb.dma("sp", comb_d.t[:, :], rt["comb"][:].rearrange("p c e -> p (c e)"), reads=[rt["comb"].r()], writes=[comb_d.r()])

    def ffn_phase(l, ntiles, passes):
        with kb.scope():
            h2T = kb.sb("h2T", [128, 8, NTOK], BF16)
            for ti in range(ntiles):
                t0, w, isc = TILES[ti]
                kb.dma("sp", h2T[:, :, t0:t0 + w], h2_d.t[:, :, t0:t0 + w], reads=[h2_d.r(ti)], writes=[h2T.r(ti)])
            wbufs = [(kb.sb("wgt", [128, 8, 896], BF16), kb.sb("wut", [128, 8, 896], BF16), kb.sb("wdt", [128, 7, D], BF16)) for _ in range(2)]
            NXO = 6
            xo = [kb.sb("fxo", [128, 512], F32) for _ in range(NXO)]
            xn = [kb.sb("fxn", [128, 512], F32) for _ in range(NXO)]
            a_b = [kb.sb("fa", [128, 7, 512], BF16) for _ in range(2)]
            sgb = [kb.sb("fsg", [128, 512], BF16) for _ in range(2)]

            def load_w(pi):
                wg_src, wu_src, wd_src, ff0, nff, ex = passes[pi]
                wgt, wut, wdt = wbufs[pi % 2]
                kb.dma("pool", wgt[:, :, :nff * 128], wg_src[:, :, ff0 * 128:(ff0 + nff) * 128], writes=[wgt.r()])
                kb.dma("pool", wut[:, :, :nff * 128], wu_src[:, :, ff0 * 128:(ff0 + nff) * 128], writes=[wut.r()])
                kb.dma("pool", wdt[:, :nff, :], wd_src[ff0 * 128:(ff0 + nff) * 128, :].rearrange("(j p) n -> p j n", p=128), writes=[wdt.r()])

            load_w(0)
            items = [(pi, ti, oc) for pi in range(len(passes)) for ti in range(ntiles) for oc in range(KC)]
            PF = 3
            nload = [0]

            def ensure_loaded(upto):
                while nload[0] < min(upto, len(items)):
                    n = nload[0]
                    _, ti_, oc_ = items[n]
                    t0_, w_, _ = TILES[ti_]
                    kb.dma("sp", xo[n % NXO][:, :w_], xTv[:, oc_, t0_:t0_ + w_], reads=[xT_d.r((ti_, oc_))], writes=[xo[n % NXO].r()])
                    nload[0] += 1

            cnt = 0
            for pi, (wg_src, wu_src, wd_src, ff0, nff, expert) in enumerate(passes):
                final = False
                wgt, wut, wdt = wbufs[pi % 2]
                for ti in range(ntiles):
                    if ti == 1 and pi + 1 < len(passes):
                        load_w(pi + 1)
                    t0, w, isc = TILES[ti]
                    wh = 1 if isc else 0
                    a_ = a_b[(pi * ntiles + ti) % 2]
                    for j in range(nff):
                        pg, pu = PB[(j % 2) * 2], PB[(j % 2) * 2 + 1]
                        for k in range(KC):
                            mm(pg[:, :w], wgt[:, k, j * 128:(j + 1) * 128], h2T[:, k, t0:t0 + w], k == 0, k == KC - 1, [wgt.r(), h2T.r(ti)], [pg.r()])
                        for k in range(KC):
                            mm(pu[:, :w], wut[:, k, j * 128:(j + 1) * 128], h2T[:, k, t0:t0 + w], k == 0, k == KC - 1, [wut.r(), h2T.r(ti)], [pu.r()])
                        sg = sgb[j % 2]
                        act(sg[:, :w], pg[:, :w], AF.Silu, [pg.r()], [sg.r()])
                        tt("dve", a_[:, j, :w], pu[:, :w], sg[:, :w], ALU.mult, [pu.r(), sg.r()], [a_.r(j)])
                    for oc in range(KC):
                        ensure_loaded(cnt + 1 + PF)
                        xo_, xn_ = xo[cnt % NXO], xn[cnt % NXO]
                        cnt += 1
                        ps = PB[4 + oc % 2]
                        for j in range(nff):
                            mm(ps[:, :w], wdt[:, j, oc * 128:(oc + 1) * 128], a_[:, j, :w], j == 0, j == nff - 1, [wdt.r(), a_.r(j)], [ps.r()])
                        stt(xn_[:, :w], ps[:, :w], aff[:, 5, oc, wh:wh + 1], xo_[:, :w], ALU.mult, ALU.add, [ps.r(), aff.r(), xo_.r()], [xn_.r()])
                        kb.dma("sp", xTv[:, oc, t0:t0 + w], xn_[:, :w], reads=[xn_.r()], writes=[xT_d.r((ti, oc))])

    def moe_sparse_phase():
        mg_rows = mg_d.t[0].rearrange("e r (q n) -> (e r q) n", n=896)
        mu_rows = mu_d.t[0].rearrange("e r (q n) -> (e r q) n", n=896)
        md_rows = md_d.t[0].rearrange("e r n -> (e r) n")
        IOA = bass.IndirectOffsetOnAxis
        with kb.scope():
            idx_hi = kb.sb("idx_hi", [128, 32], I32)
            idx_lo = kb.sb("idx_lo", [128, 32], I32)
            chi = kb.sb("chi", [128, 32], F32)
            clo = kb.sb("clo", [128, 32], F32)
            igu = kb.sb("igu", [128, NTM, 32], I32)
            idn = kb.sb("idn", [128, NTM, 28], I32)
            g5b = kb.sb("g5b", [128, D], F32)
            with kb.scope():
                g5r = kb.sb("g5r", [8, 128], F32)
                g5c = kb.sb("g5c", [128, 8], F32)
                cp("dve", g5c[:], modT[:, 40:48, 0], [modT.r()], [g5c.r()])
                kb.op("pe", lambda e: e.transpose(PB[0][0:8, 0:128], g5c[:], ident[:]), [g5c.r(), ident.r()], [PB[0].r()])
                cp("dve", g5r[:], PB[0][0:8, 0:128], [PB[0].r()], [g5r.r()])
                kb.dma("sp", g5_d.t.rearrange("o (k p) -> (o k) p", p=128), g5r[:], reads=[g5r.r()], writes=[g5_d.r()])
                kb.dma("sp", g5b[:], g5_d.t[0:1, :].to_broadcast([128, D]), reads=[g5_d.r()], writes=[g5b.r()])
            with kb.scope():
                ut_b = kb.sb("ut_b", [128, 128], BF16)
                thr = kb.sb("thr", [128, 8, 8], F32)
                tidx = kb.sb("tidx", [128, NTM, 8], F32)
                cgu = kb.sb("cgu", [128, 32], F32)
                cdn = kb.sb("cdn", [128, 28], F32)
                kb.dma("sp", ut_b[:], cd["ut_b"][:, :], writes=[ut_b.r()])
                kb.dma("sp", thr[:].rearrange("p a b -> p (a b)"), cd["thr"][:, :], writes=[thr.r()])
                kb.dma("sp", tidx[:].rearrange("p a b -> p (a b)"), cd["tidx"][:, :], writes=[tidx.r()])
                kb.dma("sp", cgu[:], cd["cgu"][:, :], writes=[cgu.r()])
                kb.dma("sp", cdn[:], cd["cdn"][:, :], writes=[cdn.r()])
                maskall = kb.sb("maskall2", [128, 32, 8], BF16)
                comball = kb.sb("comball2", [128, 32, 8], F32)
                kb.dma("sp", maskall[:].rearrange("p c e -> p (c e)"), mask_d.t[:, :], reads=[mask_d.r()], writes=[maskall.r()])
                kb.dma("sp", comball[:].rearrange("p c e -> p (c e)"), comb_d.t[:, :], reads=[comb_d.r()], writes=[comball.r()])
                mask_b = maskall
                cnt = kb.sb("cnt", [128, 32, 8], F32)
                cum = kb.sb("cum", [128, 33, 8], F32)
                pos = kb.sb("pos", [128, 32, 8], F32)
                cmpt = kb.sb("cmpt", [128, 8, 8], F32)
                ntile = kb.sb("ntile", [128, 8], F32)
                cumt = kb.sb("cumt", [128, 8], F32)
                base = kb.sb("base", [128, 8], F32)
                vals = kb.sb("vals", [128, 32, 8], F32)
                vals2 = kb.sb("vals2", [128, 32, 8], F32)
                eq = kb.sb("eq", [128, 32, 8], F32)
                hi = kb.sb("hi", [128, 32], F32)
                lo2 = kb.sb("lo2", [128, 32], F32)
                tf = kb.sb("tf", [128, 32], F32)
                cmp2 = kb.sb("cmp2", [128, NTM, 8], F32)
                etf = kb.sb("etf", [128, NTM], F32)
                fgu = kb.sb("fgu", [128, NTM, 32], F32)
                fdn = kb.sb("fdn", [128, NTM, 28], F32)

                def red(out, in_, op, rd, wr):
                    kb.op("dve", lambda e: e.tensor_reduce(out=out, in_=in_, axis=AX.X, op=op), rd, wr)

                bc, bp = PB[0], PB[1]
                mm(bc[:, 0:256], ones_b[:], mask_b[:].rearrange("p c e -> p (c e)"), True, True, [ones_b.r(), mask_b.r()], [bc.r()])
                for c in range(32):
                    mm(bp[:, c * 8:(c + 1) * 8], ut_b[:], mask_b[:, c, :], True, True, [ut_b.r(), mask_b.r()], [bp.r()])
                cp("dve", cnt[:].rearrange("p c e -> p (c e)"), bc[:, 0:256], [bc.r()], [cnt.r()])
                kb.op("dve", lambda e: e.memset(cum[:, 0, :], 0.0), [], [cum.r()])
                for c in range(32):
                    tt("dve", cum[:, c + 1, :], cum[:, c, :], cnt[:, c, :], ALU.add, [cum.r(), cnt.r()], [cum.r()])
                tt("dve", pos[:].rearrange("p c e -> p (c e)"), bp[:, 0:256], cum[:, 0:32, :].rearrange("p c e -> p (c e)"), ALU.add,
                   [bp.r(), cum.r()], [pos.r()])
                tt("dve", cmpt[:], thr[:], cum[:, 32, :].unsqueeze(2).to_broadcast([128, 8, 8]), ALU.is_lt, [thr.r(), cum.r()], [cmpt.r()])
                red(ntile[:], cmpt[:], ALU.add, [cmpt.r()], [ntile.r()])
                cp("dve", cumt[:, 0:1], ntile[:, 0:1], [ntile.r()], [cumt.r()])
                for e_ in range(1, 8):
                    tt("dve", cumt[:, e_:e_ + 1], cumt[:, e_ - 1:e_], ntile[:, e_:e_ + 1], ALU.add, [cumt.r(), ntile.r()], [cumt.r()])
                tt("dve", base[:], cumt[:], ntile[:], ALU.subtract, [cumt.r(), ntile.r()], [base.r()])
                ts("dve", base[:], base[:], 512.0, None, ALU.mult, None, [base.r()], [base.r()])
                tt("dve", vals[:], pos[:], base[:].unsqueeze(1).to_broadcast([128, 32, 8]), ALU.add, [pos.r(), base.r()], [vals.r()])
                stt(vals[:], vals[:], 1.0, maskall[:], ALU.add, ALU.mult, [vals.r(), maskall.r()], [vals.r()])
                red(hi[:], vals[:], ALU.max, [vals.r()], [hi.r()])
                stt(vals2[:], maskall[:], BIG, vals[:], ALU.mult, ALU.subtract, [vals.r(), maskall.r()], [vals2.r()])
                red(lo2[:], vals2[:], ALU.max, [vals2.r()], [lo2.r()])
                ts("dve", tf[:], hi[:], -1.0, None, ALU.add, None, [hi.r()], [tf.r()])
                cp("dve", idx_hi[:], tf[:], [tf.r()], [idx_hi.r()])
                ts("dve", tf[:], lo2[:], -1.0, BIG - 1.0, ALU.mult, ALU.add, [lo2.r()], [tf.r()])
                cp("dve", idx_lo[:], tf[:], [tf.r()], [idx_lo.r()])
                tt("dve", eq[:], vals[:], hi[:].unsqueeze(2).to_broadcast([128, 32, 8]), ALU.is_equal, [vals.r(), hi.r()], [eq.r()])
                tt("dve", eq[:], eq[:], comball[:], ALU.mult, [eq.r(), comball.r()], [eq.r()])
                red(chi[:], eq[:], ALU.add, [eq.r()], [chi.r()])
                tt("dve", eq[:], vals2[:], lo2[:].unsqueeze(2).to_broadcast([128, 32, 8]), ALU.is_equal, [vals2.r(), lo2.r()], [eq.r()])
                tt("dve", eq[:], eq[:], comball[:], ALU.mult, [eq.r(), comball.r()], [eq.r()])
                red(clo[:], eq[:], ALU.add, [eq.r()], [clo.r()])
                tt("dve", cmp2[:], tidx[:], cumt[:].unsqueeze(1).to_broadcast([128, NTM, 8]), ALU.is_ge, [tidx.r(), cumt.r()], [cmp2.r()])
                red(etf[:], cmp2[:], ALU.add, [cmp2.r()], [etf.r()])
                ts("dve", etf[:], etf[:], 7.0, None, ALU.min, None, [etf.r()], [etf.r()])
                stt(fgu[:], etf[:].unsqueeze(2).to_broadcast([128, NTM, 32]), 4096.0, cgu[:].unsqueeze(1).to_broadcast([128, NTM, 32]),
                    ALU.mult, ALU.add, [etf.r(), cgu.r()], [fgu.r()])
                cp("dve", igu[:], fgu[:], [fgu.r()], [igu.r()])
                stt(fdn[:], etf[:].unsqueeze(2).to_broadcast([128, NTM, 28]), 3584.0, cdn[:].unsqueeze(1).to_broadcast([128, NTM, 28]),
                    ALU.mult, ALU.add, [etf.r(), cdn.r()], [fdn.r()])
                cp("dve", idn[:], fdn[:], [fdn.r()], [idn.r()])
                dbg_dump("r_idx_hi", idx_hi[:], [128, 32], I32)
                dbg_dump("r_idx_lo", idx_lo[:], [128, 32], I32)
                dbg_dump("r_chi", chi[:], [128, 32], F32)
                dbg_dump("r_clo", clo[:], [128, 32], F32)
                dbg_dump("r_igu", igu[:], [128, NTM, 32], I32)
                dbg_dump("r_mask", maskall[:], [128, 32, 8], BF16)
            with kb.scope():
                wgu = [(kb.sb("wgt", [128, 8, 896], BF16), kb.sb("wut", [128, 8, 896], BF16)) for _ in range(2)]
                wd = kb.sb("wdt", [128, 28, D], BF16)
                a_ = kb.sb("fa", [128, 28, 512], BF16)
                hTt = [kb.sb("hTt", [128, 8, 512], BF16) for _ in range(2)]
                hsb = [kb.sb("hsb", [128, D], BF16) for _ in range(4)]
                sgb = [kb.sb("fsg", [128, 512], BF16) for _ in range(2)]
                ytb = [kb.sb("yt", [128, D], F32) for _ in range(2)]
                pbf6 = PB[6].t[:, :].bitcast(BF16)
                sc_res = []
                for c in range(32):
                    hs = hsb[c % 4]
                    kb.dma("sp", hs[:], h2tm_d.t[c * 128:(c + 1) * 128, :], reads=[h2tm_d.r(c)], writes=[hs.r()])
                    for nm, ix in (("h", idx_hi), ("l", idx_lo)):
                        kb.idma(hslot_d.t[:, :], IOA(ap=ix[:, c:c + 1], axis=0), hs[:], None, reads=[hs.r(), ix.r(), hslot_d.r()],
                                writes=[hslot_d.r((nm, c))])
                        sc_res.append(hslot_d.r((nm, c)))

                def load_wgu(i, q):
                    wgt, wut = wgu[(4 * i + q) % 2]
                    for k in range(KC):
                        col = k * 4 + q
                        kb.idma(wgt[:, k, :], None, mg_rows, IOA(ap=igu[:, i, col:col + 1], axis=0), reads=[igu.r()], writes=[wgt.r(k)])
                        kb.idma(wut[:, k, :], None, mu_rows, IOA(ap=igu[:, i, col:col + 1], axis=0), reads=[igu.r()], writes=[wut.r(k)])

                def load_wd(i):
                    for j in range(28):
                        kb.idma(wd[:, j, :], None, md_rows, IOA(ap=idn[:, i, j:j + 1], axis=0), reads=[idn.r()], writes=[wd.r(j)])

                load_wgu(0, 0)
                load_wgu(0, 1)
                load_wd(0)
                nev = 0
                def fetch_tile(i):
                    hTn = hTt[i % 2]
                    for c4 in range(4):
                        hs = hsb[c4]
                        s0 = i * 512 + c4 * 128
                        kb.dma("sp", hs[:], hslot_d.t[s0:s0 + 128, :], reads=sc_res if i == 0 else [], writes=[hs.r()])
                    for c4 in range(4):
                        hs = hsb[c4]
                        for k in range(KC):
                            kb.op("pe", lambda e: e.transpose(pbf6[:, k * 128:(k + 1) * 128], hs[:, k * 128:(k + 1) * 128], ident_b[:]),
                                  [hs.r(), ident_b.r()], [PB[6].r()])
                        cp("dve", hTn[:, :, c4 * 128:(c4 + 1) * 128], pbf6[:, :].rearrange("p (a b) -> p a b", b=128), [PB[6].r()], [hTn.r()])

                for i in range(NTM):
                    hT_ = hTt[i % 2]
                    fetch_tile(i)
                    for q in range(4):
                        wgt, wut = wgu[(4 * i + q) % 2]
                        for j in range(7):
                            jj = q * 7 + j
                            pg, pu = PB[(j % 2) * 2], PB[(j % 2) * 2 + 1]
                            for k in range(KC):
                                mm(pg[:, :], wgt[:, k, j * 128:(j + 1) * 128], hT_[:, k, :], k == 0, k == KC - 1, [wgt.r(k), hT_.r()], [pg.r()])
                            for k in range(KC):
                                mm(pu[:, :], wut[:, k, j * 128:(j + 1) * 128], hT_[:, k, :], k == 0, k == KC - 1, [wut.r(k), hT_.r()], [pu.r()])
                            sg = sgb[j % 2]
                            act(sg[:], pg[:, :], AF.Silu, [pg.r()], [sg.r()])
                            tt("dve", a_[:, jj, :], pu[:, :], sg[:], ALU.mult, [pu.r(), sg.r()], [a_.r(jj)])
                        if q < 2:
                            load_wgu(i, q + 2)
                        elif i + 1 < NTM:
                            load_wgu(i + 1, q - 2)
                    for sc in range(4):
                        yt = ytb[sc % 2]
                        for fh in range(2):
                            ps = PB[4 + fh]
                            for jj in range(28):
                                mm(ps[:, :], a_[:, jj, sc * 128:(sc + 1) * 128], wd[:, jj, fh * 512:(fh + 1) * 512], jj == 0, jj == 27,
                                   [a_.r(jj), wd.r(jj)], [ps.r()])
                            cp("act" if nev % 2 == 0 else "dve", yt[:, fh * 512:(fh + 1) * 512], ps[:, :], [ps.r()], [yt.r()])
                            nev += 1
                        s0 = i * 512 + sc * 128
                        kb.dma("sp", yslot_d.t[s0:s0 + 128, :], yt[:], reads=[yt.r()], writes=[yslot_d.r((i, sc))])
                    if i + 1 < NTM:
                        load_wd(i + 1)
            with kb.scope():
                ys_res = [yslot_d.r((i, sc)) for i in range(NTM) for sc in range(4)]
                NB = 6
                ehb = [kb.sb("ehb", [128, D], F32) for _ in range(NB)]
                elb = [kb.sb("elb", [128, D], F32) for _ in range(NB)]
                xcb = [kb.sb("xcb", [128, D], F32) for _ in range(NB)]
                ocb = [kb.sb("ocb", [128, D], F32) for _ in range(NB)]
                for c in range(32):
                    eh, el, xc, oc_ = ehb[c % NB], elb[c % NB], xcb[c % NB], ocb[c % NB]
                    kb.dma("sp", xc[:], xtm_d.t[c * 128:(c + 1) * 128, :], reads=[xtm_d.r(c)], writes=[xc.r()])
                    kb.idma(eh[:], None, yslot_d.t[:, :], IOA(ap=idx_hi[:, c:c + 1], axis=0), reads=[idx_hi.r()] + (ys_res if c == 0 else []), writes=[eh.r()])
                    kb.idma(el[:], None, yslot_d.t[:, :], IOA(ap=idx_lo[:, c:c + 1], axis=0), reads=[idx_lo.r()], writes=[el.r()])
                    act(eh[:], eh[:], AF.Copy, [eh.r(), chi.r()], [eh.r()], scale=chi[:, c:c + 1])
                    stt(el[:], el[:], clo[:, c:c + 1], eh[:], ALU.mult, ALU.add, [el.r(), clo.r(), eh.r()], [el.r()])
                    tt("dve", el[:], el[:], g5b[:], ALU.mult, [el.r(), g5b.r()], [el.r()])
                    tt("dve", oc_[:], el[:], xc[:], ALU.add, [el.r(), xc.r()], [oc_.r()])
                    kb.dma("sp", out_d.t[c * 128:(c + 1) * 128, :], oc_[:], reads=[oc_.r()], writes=[out_d.r(c)])

    marks = []

    def mark(name):
        marks.append((name, dict((k, v) for k, v in kb.cnt.items() if isinstance(k, str))))

    kb.marks = marks

    def layer(l):
        last = (l == 1)
        ntile_res = 8 if last else 9
        mark("mod%d" % l)
        mod_phase(l)
        dbg_dump("aff%d" % l, aff[:], [128, 6, 8, 2], F32)
        with kb.scope():
            ycatT = kb.sb("ycatT", [128, 8, NTOK], BF16)
            ztc = kb.sb("ztc", [128, 2, 384], BF16)
            ztm = Tl(ycatT.t[:, 0:3, :].rearrange("p k t -> p (k t)")[:, 0:32 * 384].rearrange("p (a b) -> p a b", b=384))
            with kb.scope():
                hT = kb.sb("hT", [128, 8, NTOK], BF16)
                mark("normA%d" % l)
                normA_phase(l, hT)
                dbg_dump("hT%d" % l, hT[:], [128, 8, NTOK], BF16)
                if stop == "normA":
                    return True
                mark("attn%d" % l)
                attn_groups(l, hT, ycatT)
                if stop == "proj":
                    return True
                dbg_dump("yatt%d" % l, ycatT[:], [128, 8, NTOK], BF16)
                if stop == "attn":
                    return True
                mark("hyproj%d" % l)
                hyena_proj(l, hT, ztm, ztc)
            dbg_dump("ztm%d" % l, ztm[:], [128, 32, 384], BF16)
            mark("hydft%d" % l)
            hyena_dft(l, S, ztm, ycatT, 0)
            mark("hydftc%d" % l)
            if not last:
                hyena_dft(l, LC, ztc, ycatT, S)
            dbg_dump("ycat_raw%d" % l, ycatT[:], [128, 8, NTOK], BF16)
            mark("gnorm%d" % l)
            group_norm(l, ycatT, ntile_res)
            dbg_dump("ycat%d" % l, ycatT[:], [128, 8, NTOK], BF16)
            mark("wout%d" % l)
            wout_phase(l, ycatT, ntile_res, moe=last)
        dbg_dump("xmix%d" % l, xT_d.t[:, :], [D, NTOK], F32)
        if not last:
            dbg_dump("h2T%d" % l, h2_d.t[:, :, :], [128, 8, NTOK], BF16)
        if stop == "mix%d" % l:
            return True
        if not last:
            fg = fg_d.t[0].rearrange("(k p) n -> p k n", p=128)
            fu = fu_d.t[0].rearrange("(k p) n -> p k n", p=128)
            passes = [(fg, fu, fd_d.t[0], ff0, nff, None) for (ff0, nff) in ((0, 6), (6, 6), (12, 5), (17, 5))]
            mark("ffn%d" % l)
            ffn_phase(l, ntile_res, passes)
            dbg_dump("xffn%d" % l, xT_d.t[:, :], [D, NTOK], F32)
        else:
            mark("moe")
            moe_sparse_phase()
            mark("end")
        return stop == "ffn%d" % l

    for l in range(nlayers):
        if layer(l):
            break
    kb.finish()
    es.close()
    return nc, din, dbg_d, kb


def make_in_maps(inputs):
    f = lambda a: np.ascontiguousarray(np.asarray(a, np.float32))
    cst = _consts()
    sh = {}
    sh["w_mod"] = f(inputs["w_mod"])
    sh["b_mod_pk"] = np.stack([_pk(inputs["b_mod"][l], 48) for l in range(2)])
    sh["norm_pk"] = np.stack([np.concatenate([_pk(inputs["norm_mix"][l], 8), _pk(inputs["norm_ffn"][l], 8)], 1) for l in range(2)])
    sh["w_in"] = f(inputs["w_in"])
    cw = np.asarray(inputs["hy_conv_w"], np.float32)
    cbias = np.asarray(inputs["hy_conv_b"], np.float32)
    conv = []
    for l in range(2):
        arr = np.stack([_pk(cw[l, 0], 9), _pk(cw[l, 1], 9), _pk(cw[l, 2], 9), _pk(cbias[l], 9)], axis=2)
        conv.append(arr.reshape(128, 36))
    sh["conv_pk"] = np.stack(conv)
    sh["hy_w1"] = f(inputs["hy_w1"])
    sh["hy_vec"] = np.stack([np.stack([inputs["hy_freq"][l], inputs["hy_b1"][l], inputs["hy_b2"][l, 0], inputs["hy_b2"][l, 1]], 1) for l in range(2)]).astype(np.float32)
    sh["hy_w2"] = f(inputs["hy_w2"])
    sh["hy_w3"] = f(inputs["hy_w3"])
    sh["hy_skip"] = f(inputs["hy_skip"]).reshape(2, 1, 384)
    sh["qkn_pk"] = np.stack([np.stack([np.tile(np.asarray(inputs[k][l], np.float32), 2) for k in ("na_q_norm", "na_k_norm", "ga_q_norm", "ga_k_norm")], 1)
                             for l in range(2)])
    rpb = np.asarray(inputs["na_rpb"], np.float32)
    kc = np.arange(64)[:, None]
    qc = np.arange(64)[None, :]
    dc = np.clip(kc - qc, -15, 15) + 15
    sh["rpbT"] = np.ascontiguousarray(rpb[:, :, :, dc].transpose(0, 3, 1, 2, 4).reshape(2, 64, 90, 64))
    sh["onorm_pk"] = np.stack([_pk(np.concatenate([inputs["out_norm_hy"][l], inputs["out_norm_na"][l], inputs["out_norm_ga"][l]]), 8) for l in range(2)])
    sh["w_out"] = f(inputs["w_out"])
    for k in ("ffn_w_gate", "ffn_w_up", "ffn_w_down", "moe_router", "moe_w_gate", "moe_w_up", "moe_w_down"):
        sh[k] = f(inputs[k])
    for k, v in cst.items():
        sh["k_" + k] = v
    maps = []
    x = np.asarray(inputs["x"], np.float32)
    c = np.asarray(inputs["c"], np.float32)
    ctx = np.asarray(inputs["ctx"], np.float32)
    cctx = _pk(inputs["c_ctx"], 8)
    for b in range(8):
        m = dict(sh)
        m["x"] = np.ascontiguousarray(x[b])
        m["ctx"] = np.ascontiguousarray(ctx[b])
        m["c_pk"] = np.ascontiguousarray(np.concatenate([_pk(c[b], 8), cctx], 1))
        maps.append(m)
    return maps


_PROG = {}


def kernel(**inputs):
    if "p" not in _PROG:
        _PROG["p"] = build_program()
    nc, din, dbg_d, kb = _PROG["p"]
    maps = make_in_maps(inputs)
    res = run_bass_kernel_spmd(nc, maps, core_ids=list(range(8)))
    out = np.stack([np.asarray(res.results[b]["out"], np.float32) for b in range(8)])
    return out
```

```python
import math
from contextlib import ExitStack, contextmanager
import numpy as np
import ml_dtypes
import concourse.bass as bass
import concourse.mybir as mybir
from concourse.bass_utils import run_bass_kernel_spmd

F32 = mybir.dt.float32
BF16 = mybir.dt.bfloat16
AF = mybir.ActivationFunctionType
ALU = mybir.AluOpType
AX = mybir.AxisListType

D = 1024
KC = 8
S = 4096
LC = 256
NTOK = S + LC
GW = 64
DHY = 384
DFF = 2816
NE = 8
DFE = 3584
EPS = 1e-6
PI = math.pi
NTM = 23
NSLOT = NTM * 512
BIG = 16384.0
I32 = mybir.dt.int32


class Res:
    __slots__ = ("w", "r")

    def __init__(self):
        self.w = None
        self.r = {}


class Tl:
    def __init__(self, t):
        self.t = t
        self._res = {}

    def r(self, key=None):
        x = self._res.get(key)
        if x is None:
            x = self._res[key] = Res()
        return x

    def __getitem__(self, idx):
        return self.t[idx]


class KB:
    ENG = ("pe", "act", "dve", "pool", "sp")

    def __init__(self, nc, es, nslots=8):
        self.nc = nc
        self.es = es
        self.eng = {"pe": nc.tensor, "act": nc.scalar, "dve": nc.vector, "pool": nc.gpsimd, "sp": nc.sync}
        self.semh = {}
        self.cnt = {}
        for e in self.ENG:
            self.semh[e] = es.enter_context(nc.semaphore("c_" + e))
            self.cnt[e] = 0
        self.seen = {e: {} for e in self.ENG}
        self.slots = {}
        self.slotpos = {}
        for q in ("sp", "pool"):
            self.slots[q] = []
            for i in range(nslots):
                k = ("d", q, i)
                self.semh[k] = es.enter_context(nc.semaphore("d_%s%d" % (q, i)))
                self.cnt[k] = 0
                self.slots[q].append(k)
            self.slotpos[q] = 0
        self.ninst = 0
        self.uid = 0

    def sb(self, name, shape, dt):
        self.uid += 1
        return Tl(self.es.enter_context(self.nc.sbuf_tensor("%s_%d" % (name, self.uid), list(shape), dt)))

    def ps(self, name, shape, dt):
        return Tl(self.es.enter_context(self.nc.psum_tensor(name, list(shape), dt)))

    def dram(self, name, shape, dt, kind):
        return Tl(self.nc.dram_tensor(name, list(shape), dt, kind=kind).ap())

    @contextmanager
    def scope(self):
        old = self.es
        with ExitStack() as s:
            self.es = s
            try:
                yield
            finally:
                self.barrier()
                self.es = old

    def barrier(self):
        for e in self.ENG:
            seen = self.seen[e]
            for k, v in self.cnt.items():
                if k != e and v > 0 and seen.get(k, 0) < v:
                    self.eng[e].wait_ge(self.semh[k], v)
                    seen[k] = v
                    self.ninst += 1

    def _deps(self, e, reads, writes):
        raw = {}
        oth = {}
        for r in reads:
            ev = r.w
            if ev is not None and raw.get(ev[0], 0) < ev[1]:
                raw[ev[0]] = ev[1]
        for w in writes:
            ev = w.w
            if ev is not None and oth.get(ev[0], 0) < ev[1]:
                oth[ev[0]] = ev[1]
            for k, v in w.r.items():
                if oth.get(k, 0) < v:
                    oth[k] = v
        for k, v in oth.items():
            if k == e and e == "pe":
                continue
            if raw.get(k, 0) < v:
                raw[k] = v
        seen = self.seen[e]
        eng = self.eng[e]
        for k, v in raw.items():
            if seen.get(k, 0) < v:
                eng.wait_ge(self.semh[k], v)
                seen[k] = v
                self.ninst += 1

    def _mark(self, ev, reads, writes):
        k, v = ev
        for r in reads:
            if r.r.get(k, 0) < v:
                r.r[k] = v
        for w in writes:
            w.w = ev
            w.r = {}

    def op(self, e, fn, reads=(), writes=()):
        self._deps(e, reads, writes)
        inst = fn(self.eng[e])
        self.cnt[e] += 1
        inst.then_inc(self.semh[e], 1)
        self.ninst += 1
        self._mark((e, self.cnt[e]), reads, writes)
        return inst

    def dma(self, q, out, in_, reads=(), writes=(), **kw):
        k = self.slots[q][self.slotpos[q]]
        self.slotpos[q] = (self.slotpos[q] + 1) % len(self.slots[q])
        self._deps(q, reads, writes)
        seen = self.seen[q]
        if seen.get(k, 0) < self.cnt[k]:
            self.eng[q].wait_ge(self.semh[k], self.cnt[k])
            seen[k] = self.cnt[k]
        inst = self.eng[q].dma_start(out=out, in_=in_, **kw)
        self.cnt[k] += 16
        inst.then_inc(self.semh[k], 16)
        self.ninst += 1
        self._mark((k, self.cnt[k]), reads, writes)
        return inst

    def idma(self, out, out_off, in_, in_off, reads=(), writes=()):
        q = "pool"
        k = self.slots[q][self.slotpos[q]]
        self.slotpos[q] = (self.slotpos[q] + 1) % len(self.slots[q])
        self._deps(q, reads, writes)
        seen = self.seen[q]
        if seen.get(k, 0) < self.cnt[k]:
            self.eng[q].wait_ge(self.semh[k], self.cnt[k])
            seen[k] = self.cnt[k]
        inst = self.nc.gpsimd.indirect_dma_start(out, out_off, in_, in_off)
        self.cnt[k] += 16
        inst.then_inc(self.semh[k], 16)
        self.ninst += 1
        self._mark((k, self.cnt[k]), reads, writes)
        return inst

    def finish(self):
        sp = self.eng["sp"]
        for k, v in self.cnt.items():
            if v > 0 and k != "sp" and self.seen["sp"].get(k, 0) < v:
                sp.wait_ge(self.semh[k], v)


def _bf(a):
    return np.ascontiguousarray(a.astype(ml_dtypes.bfloat16))


def _pk(v, nchunk):
    return np.ascontiguousarray(np.asarray(v, np.float32).reshape(nchunk, 128).T)


_CONST_CACHE = {}


def _dft_consts(L):
    t = np.arange(L, dtype=np.int64)[:, None]
    f = np.arange(L, dtype=np.int64)[None, :]
    m = ((2 * f + 1) * t) % (4 * L)
    ang = m.astype(np.float64) * (np.pi / (2 * L))
    out = {}
    nt = L // 128
    W = min(512, L)
    for nm, M in (("C", np.cos(ang)), ("S", np.sin(ang))):
        Mb = M.astype(np.float32).astype(ml_dtypes.bfloat16)
        out["FW" + nm] = np.ascontiguousarray(Mb.reshape(nt, 128, nt, 128).transpose(2, 1, 0, 3))
        out["IV" + nm] = np.ascontiguousarray(Mb.reshape(L // W, W, nt, 128).transpose(0, 3, 2, 1))
    return out


def _hy_pos(L):
    bands = 16
    t = np.linspace(0.0, 1.0, L, dtype=np.float32)[:, None]
    ang = (2.0 * np.float32(math.pi) * np.arange(L, dtype=np.float32)[:, None] / np.float32(L)).astype(np.float32)
    fr = np.linspace(1e-4, bands - 1, bands, dtype=np.float32)[None, :]
    z = np.concatenate([t, np.cos(fr * ang), -np.sin(fr * ang)], axis=-1).astype(np.float32)
    return np.ascontiguousarray(z.T), np.ascontiguousarray((-t[:, 0]).reshape(L // 128, 128).T)


def _consts():
    if _CONST_CACHE:
        return _CONST_CACHE
    c = {}
    c["ident"] = np.eye(128, dtype=np.float32)
    c["ones_b"] = _bf(np.ones((128, 128), np.float32))
    blk = np.zeros((128, 128), np.float32)
    blk[:64, :64] = 1
    blk[64:, 64:] = 1
    c["blk_b"] = _bf(blk)
    rm = np.zeros((128, 128), np.float32)
    for m in range(128):
        d = m % 32
        if d < 16:
            rm[m + 16, m] = -1.0
        else:
            rm[m - 16, m] = 1.0
    c["rm_b"] = _bf(rm)
    sel = np.zeros((8, 8, 128), np.float32)
    for e in range(8):
        sel[e, e, :] = 1.0
    c["sel"] = sel.reshape(8, 1024)
    tok = np.arange(S)
    pos_row = (tok // GW).astype(np.float32)
    pos_col = (tok % GW).astype(np.float32)
    inv = np.power(np.float32(10000.0), -np.arange(16, dtype=np.float32) / np.float32(16)).astype(np.float32)
    cosT = np.zeros((128, S), np.float32)
    sinT = np.zeros((128, S), np.float32)
    for p in range(128):
        d = p % 64
        pos = pos_row if d < 32 else pos_col
        a = pos * inv[d % 16]
        cosT[p] = np.cos(a.astype(np.float32))
        sinT[p] = np.sin(a.astype(np.float32))
    c["cosT"] = _bf(cosT)
    c["sinT"] = _bf(sinT)
    for L in (S, LC):
        zp, negt = _hy_pos(L)
        c["zpos%d" % L] = zp
        c["negt%d" % L] = negt
        for k, v in _dft_consts(L).items():
            c["%s%d" % (k, L)] = v
    deltas = np.linspace(math.log(1e-2) / 1.5, math.log(1e-2) / 0.3, DHY, dtype=np.float32)
    c["dabs"] = np.ascontiguousarray(np.tile(np.abs(deltas)[None, :], (128, 1)).astype(np.float32))
    cidx = np.arange(GW)
    cs = np.clip(cidx - 8, 0, GW - 16)
    cm = ((cidx[None, :] >= cs[:, None]) & (cidx[None, :] < cs[:, None] + 16))
    c["colmask"] = np.ascontiguousarray(np.tile(cm.T.astype(np.float32), (2, 1)))
    ut = np.triu(np.ones((128, 128), np.float32), 1)
    c["ut_b"] = _bf(ut)
    c["thr"] = np.ascontiguousarray(np.tile((512.0 * np.arange(8, dtype=np.float32))[None, None, :], (128, 8, 1)).reshape(128, 64))
    c["tidx"] = np.ascontiguousarray(np.tile(np.arange(NTM, dtype=np.float32)[None, :, None], (128, 1, 8)).reshape(128, NTM * 8))
    p = np.arange(128, dtype=np.float32)[:, None]
    kq = np.arange(32, dtype=np.float32)[None, :]
    c["cgu"] = np.ascontiguousarray(((kq // 4) * 128 + p) * 4 + (kq % 4)).astype(np.float32)
    c["cdn"] = np.ascontiguousarray(np.arange(28, dtype=np.float32)[None, :] * 128 + p).astype(np.float32)
    _CONST_CACHE.update(c)
    return c


def _na_variant(r):
    w0 = min(max(r - 4, 0), 56)
    return (w0 % 2, w0 - r)


NA_VARIANTS = sorted(set(_na_variant(r) for r in range(64)))


def build_program(dbg=(), nlayers=2, stop=None):
    nc = bass.Bass("TRN2", target_bir_lowering=False)
    es = ExitStack()
    kb = KB(nc, es, nslots=8)
    cst = _consts()
    din = {}

    def inp(name, shape, dt=F32):
        din[name] = kb.dram(name, shape, dt, "ExternalInput")
        return din[name]

    x_d = inp("x", [S, D])
    ctx_d = inp("ctx", [LC, D])
    cpk_d = inp("c_pk", [128, 16])
    wmod_d = inp("w_mod", [2, D, 6 * D])
    bmod_d = inp("b_mod_pk", [2, 128, 48])
    nrm_d = inp("norm_pk", [2, 128, 16])
    win_d = inp("w_in", [2, D, 3 * D])
    cw_d = inp("conv_pk", [2, 128, 36])
    hyw1_d = inp("hy_w1", [2, 33, 64])
    hyv_d = inp("hy_vec", [2, 64, 4])
    hyw2_d = inp("hy_w2", [2, 2, 64, 64])
    hyw3_d = inp("hy_w3", [2, 64, 768])
    hysk_d = inp("hy_skip", [2, 1, 384])
    qkn_d = inp("qkn_pk", [2, 128, 4])
    rpb_d = inp("rpbT", [2, 64, 90, 64])
    onrm_d = inp("onorm_pk", [2, 128, 8])
    wout_d = inp("w_out", [2, D, D])
    fg_d = inp("ffn_w_gate", [1, D, DFF])
    fu_d = inp("ffn_w_up", [1, D, DFF])
    fd_d = inp("ffn_w_down", [1, DFF, D])
    mr_d = inp("moe_router", [1, D, NE])
    mg_d = inp("moe_w_gate", [1, NE, D, DFE])
    mu_d = inp("moe_w_up", [1, NE, D, DFE])
    md_d = inp("moe_w_down", [1, NE, DFE, D])
    cd = {}
    for k, v in cst.items():
        cd[k] = inp("k_" + k, list(v.shape), BF16 if v.dtype == ml_dtypes.bfloat16 else F32)
    out_d = kb.dram("out", [S, D], F32, "ExternalOutput")
    dbg_d = {}
    xT_d = kb.dram("xT_scr", [D, NTOK], F32, "Internal")
    x0c_d = kb.dram("x0c_scr", [128, 3, NTOK], BF16, "Internal")
    h2_d = kb.dram("h2_scr", [128, 8, NTOK], BF16, "Internal")
    h2tm_d = kb.dram("h2tm_scr", [S, D], BF16, "Internal")
    xtm_d = kb.dram("xtm_scr", [S, D], F32, "Internal")
    hslot_d = kb.dram("hslot_scr", [NSLOT, D], BF16, "Internal")
    yslot_d = kb.dram("yslot_scr", [NSLOT, D], F32, "Internal")
    g5_d = kb.dram("g5_scr", [1, D], F32, "Internal")
    xTv = xT_d.t.rearrange("(k p) t -> p k t", p=128)

    def dbg_dump(name, ap, shape, dt):
        if name in dbg:
            dbg_d[name] = kb.dram("dbg_" + name, shape, dt, "ExternalOutput")
            kb.barrier()
            kb.dma("sp", dbg_d[name].t, ap)
            kb.barrier()

    PB = [kb.ps("bank%d" % i, [128, 512], F32) for i in range(8)]

    def mm(out, lhsT, rhs, start, stop, reads, writes):
        kb.op("pe", lambda e: e.matmul(out, lhsT, rhs, start=start, stop=stop), reads, writes)

    def act(out, in_, func, reads, writes, **kw):
        kb.op("act", lambda e: e.activation(out=out, in_=in_, func=func, **kw), reads, writes)

    def tt(eng, out, in0, in1, op, reads, writes):
        kb.op(eng, lambda e: e.tensor_tensor(out=out, in0=in0, in1=in1, op=op), reads, writes)

    def ts(eng, out, in0, s1, s2, op0, op1, reads, writes):
        if op1 is None:
            kb.op(eng, lambda e: e.tensor_scalar(out=out, in0=in0, scalar1=s1, scalar2=None, op0=op0), reads, writes)
        else:
            kb.op(eng, lambda e: e.tensor_scalar(out=out, in0=in0, scalar1=s1, scalar2=s2, op0=op0, op1=op1), reads, writes)

    def stt(out, in0, scalar, in1, op0, op1, reads, writes):
        kb.op("dve", lambda e: e.scalar_tensor_tensor(out=out, in0=in0, scalar=scalar, in1=in1, op0=op0, op1=op1), reads, writes)

    def cp(eng, out, in_, reads, writes):
        if eng == "act":
            act(out, in_, AF.Copy, reads, writes)
        else:
            kb.op(eng, lambda e: e.tensor_copy(out=out, in_=in_), reads, writes)

    def recip(out, in_, reads, writes):
        kb.op("dve", lambda e: e.reciprocal(out=out, in_=in_), reads, writes)

    ident = kb.sb("ident", [128, 128], F32)
    kb.dma("sp", ident[:], cd["ident"][:, :], writes=[ident.r()])
    ident_b = kb.sb("identb", [128, 128], BF16)
    kb.dma("pool", ident_b[:], cd["ident"][:, :], writes=[ident_b.r()])
    ones_b = kb.sb("ones_b", [128, 128], BF16)
    kb.dma("sp", ones_b[:], cd["ones_b"][:, :], writes=[ones_b.r()])
    blk_b = kb.sb("blk_b", [128, 128], BF16)
    kb.dma("sp", blk_b[:], cd["blk_b"][:, :], writes=[blk_b.r()])
    rm_b = kb.sb("rm_b", [128, 128], BF16)
    kb.dma("sp", rm_b[:], cd["rm_b"][:, :], writes=[rm_b.r()])
    cpk = kb.sb("cpk", [128, 16], F32)
    kb.dma("sp", cpk[:], cpk_d[:, :], writes=[cpk.r()])
    sc_b = kb.sb("sc_b", [128, 8, 2], BF16)
    act(sc_b[:, :, 0], cpk[:, 0:8], AF.Silu, [cpk.r()], [sc_b.r()])
    act(sc_b[:, :, 1], cpk[:, 8:16], AF.Silu, [cpk.r()], [sc_b.r()])
    modT = kb.sb("modT", [128, 48, 2], F32)
    aff = kb.sb("aff", [128, 6, 8, 2], F32)
    prm = kb.sb("prm", [128, 2, 72], F32)
    bmod = kb.sb("bmod", [128, 2, 48], F32)
    wr32 = kb.sb("wr32", [128, 8, 8], F32)
    kb.dma("sp", wr32[:], mr_d.t[0].rearrange("(k p) n -> p k n", p=128), writes=[wr32.r()])
    lg = kb.sb("lg", [128, 40], F32)
    rt = {}
    mask_d = kb.dram("mask_scr", [128, 256], BF16, "Internal")
    comb_d = kb.dram("comb_scr", [128, 256], F32, "Internal")
    with kb.scope():
        zt = kb.sb("zt", [128, 4096], BF16)
        kb.op("pool", lambda e: e.memset(zt[:], 0.0), [], [zt.r()])
        hv = hslot_d.t.rearrange("(p a) n -> p (a n)", p=128)
        for i in range(NSLOT * D // 128 // 4096):
            kb.dma("sp", hv[:, i * 4096:(i + 1) * 4096], zt[:], reads=[zt.r()], writes=[hslot_d.r()])
    for l in range(2):
        kb.dma("sp", prm[:, l, 0:16], nrm_d[l, :, :], writes=[prm.r()])
        kb.dma("sp", prm[:, l, 16:52], cw_d[l, :, :], writes=[prm.r()])
        kb.dma("sp", prm[:, l, 52:56], qkn_d[l, :, :], writes=[prm.r()])
        kb.dma("sp", prm[:, l, 56:64], onrm_d[l, :, :], writes=[prm.r()])
        kb.dma("sp", bmod[:, l, :], bmod_d[l, :, :], writes=[bmod.r()])
    for l in range(2):
        ts("dve", prm[:, l, 64:65], prm[:, l, 52:53], 0.125, None, ALU.mult, None, [prm.r()], [prm.r()])
        ts("dve", prm[:, l, 65:66], prm[:, l, 54:55], 0.125, None, ALU.mult, None, [prm.r()], [prm.r()])

    TILES = [(i * 512, 512, False) for i in range(8)] + [(S, LC, True)]

    def mod_phase(l):
        with kb.scope():
            wm = [kb.sb("wm", [128, 8, 512], BF16) for _ in range(2)]
            bank = PB[0]
            src = wmod_d.t[l].rearrange("(k p) n -> p k n", p=128)
            for cb in range(12):
                w = wm[cb % 2]
                kb.dma("pool", w[:], src[:, :, cb * 512:(cb + 1) * 512], writes=[w.r()])
                for j in range(4):
                    oc = cb * 4 + j
                    for k in range(KC):
                        mm(bank[:, oc * 2:oc * 2 + 2], w[:, k, j * 128:(j + 1) * 128], sc_b[:, k, :], k == 0, k == KC - 1,
                           [w.r(), sc_b.r()], [bank.r()])
            tt("dve", modT[:], bank[:, 0:96].rearrange("p (a b) -> p a b", b=2),
               bmod[:, l, :].unsqueeze(2).to_broadcast([128, 48, 2]), ALU.add, [bank.r(), bmod.r()], [modT.r()])
            for (dst, gi, mi) in ((0, 0, 1), (3, 8, 4)):
                ts("dve", aff[:, dst], modT[:, mi * 8:(mi + 1) * 8, :], 1.0, None, ALU.add, None, [modT.r()], [aff.r()])
                tt("dve", aff[:, dst], aff[:, dst], prm[:, l, gi:gi + 8].unsqueeze(2).to_broadcast([128, 8, 2]), ALU.mult,
                   [aff.r(), prm.r()], [aff.r()])
            for (dst, mi) in ((1, 0), (2, 2), (4, 3), (5, 5)):
                cp("dve", aff[:, dst], modT[:, mi * 8:(mi + 1) * 8, :], [modT.r()], [aff.r()])

    def rms_tile(w, n_feat, chunks, pb, tmp):
        sq = tmp["sq"]
        n = len(chunks)
        for i, (ap, rd) in enumerate(chunks):
            act(sq[:, i % 2, :w], ap, AF.Square, rd, [sq.r(i % 2)])
            mm(pb[:, :w], ones_b[:], sq[:, i % 2, :w], i == 0, i == n - 1, [ones_b.r(), sq.r(i % 2)], [pb.r()])
        act(tmp["rt"][:, :w], pb[:, :w], AF.Ln, [pb.r()], [tmp["rt"].r()], scale=1.0 / n_feat, bias=EPS)
        act(tmp["R"][:, :w], tmp["rt"][:, :w], AF.Exp, [tmp["rt"].r()], [tmp["R"].r()], scale=-0.5)

    def norm_tile(l, sub, xt, ti, dst, tmp, h32=None):
        t0, w, isc = TILES[ti]
        wh = 1 if isc else 0
        ai, bi = (0, 1) if sub == 0 else (3, 4)
        rms_tile(w, D, [(xt[:, k, :w], [xt.r()]) for k in range(KC)], PB[7], tmp)
        for k in range(KC):
            u = tmp["u"]
            stt(u[:, k % 2, :w], xt[:, k, :w], aff[:, ai, k, wh:wh + 1], tmp["R"][:, :w], ALU.mult, ALU.mult,
                [xt.r(), aff.r(), tmp["R"].r()], [u.r(k % 2)])
            oap, ores = dst(k)
            act(oap, u[:, k % 2, :w], AF.Identity, [u.r(k % 2), aff.r()], ores, bias=aff[:, bi, k, wh:wh + 1])
            if h32 is not None:
                ts("pool", h32[:, k, :w], u[:, k % 2, :w], 1.0, aff[:, bi, k, wh:wh + 1], ALU.mult, ALU.add, [u.r(k % 2), aff.r()], [h32.r()])

    def norm_tmp():
        return {"sq": kb.sb("sq", [128, 2, 512], BF16), "rt": kb.sb("rt", [128, 512], F32), "R": kb.sb("R", [128, 512], F32),
                "u": kb.sb("u", [128, 2, 512], F32)}

    def normA_phase(l, hT):
        with kb.scope():
            tmp = norm_tmp()
            xts = [kb.sb("xt", [128, 8, 512], F32) for _ in range(2)]
            xin = [kb.sb("xin", [128, D], F32) for _ in range(3)]
            nin = 0
            st_ = {"nin": 0}

            def T_(ti):
                t0, w, isc = TILES[ti]
                xt = xts[ti % 2]
                if l == 0:
                    for c4 in range(w // 128):
                        xi = xin[st_["nin"] % 3]
                        st_["nin"] += 1
                        src = ctx_d.t[c4 * 128:(c4 + 1) * 128, :] if isc else x_d.t[t0 + c4 * 128:t0 + (c4 + 1) * 128, :]
                        kb.dma("sp", xi[:], src, writes=[xi.r()])
                        for kh in range(2):
                            pb = PB[(c4 * 2 + kh) % 4]
                            for kk in range(4):
                                k = kh * 4 + kk
                                kb.op("pe", lambda e: e.transpose(pb[:, kk * 128:(kk + 1) * 128], xi[:, k * 128:(k + 1) * 128], ident[:]),
                                      [xi.r(), ident.r()], [pb.r()])
                            cp("act" if kh == 0 else "dve", xt[:, kh * 4:(kh + 1) * 4, c4 * 128:(c4 + 1) * 128],
                               pb[:, :].rearrange("p (a b) -> p a b", b=128), [pb.r()], [xt.r()])
                    kb.dma("sp", xTv[:, :, t0:t0 + w], xt[:, :, :w], reads=[xt.r()], writes=[xT_d.r(ti)])
                else:
                    kb.dma("sp", xt[:, :, :w], xTv[:, :, t0:t0 + w], reads=[xT_d.r(ti)], writes=[xt.r()])

            T_(0)
            for ti, (t0, w, isc) in enumerate(TILES):
                if ti + 1 < len(TILES):
                    T_(ti + 1)
                norm_tile(l, 0, xts[ti % 2], ti, lambda k, t0=t0, w=w, ti=ti: (hT[:, k, t0:t0 + w], [hT.r(ti)]), tmp)

    def attn_groups(l, hT, ycatT):
        src_w = win_d.t[l].rearrange("(k p) n -> p k n", p=128)
        with kb.scope():
            qT = kb.sb("qT", [128, NTOK], BF16)
            kT = kb.sb("kT", [128, NTOK], BF16)
            vtm = kb.sb("vtm", [128, 34, 128], BF16)
            wgs = [kb.sb("wg", [128, 8, 384], BF16) for _ in range(2)]
            E = [kb.sb("E", [128, 512], BF16) for _ in range(4)]
            GA0 = 3 * (DHY + 384)
            NA0 = 3 * DHY
            PCOLS = [(GA0 + gp * 128, GA0 + 256 + gp * 128, GA0 + 512 + gp * 128) for gp in range(2)] + \
                    [(NA0 + np_ * 128, NA0 + 384 + np_ * 128, NA0 + 768 + np_ * 128) for np_ in range(3)]
            pstate = {"n": 0}

            def load_wg(idx):
                if idx >= len(PCOLS):
                    return
                for j, c0 in enumerate(PCOLS[idx]):
                    kb.dma("pool", wgs[idx % 2][:, :, j * 128:(j + 1) * 128], src_w[:, :, c0:c0 + 128], writes=[wgs[idx % 2].r(j)])

            load_wg(0)
            tmp = {"sq2": [kb.sb("sqa", [128, 512], BF16) for _ in range(2)], "rt": kb.sb("rta", [128, 512], F32), "R": kb.sb("Ra", [128, 512], F32),
                   "Rd": [kb.sb("Rd", [128, 512], F32) for _ in range(1)]}
            state = {"ob": 0, "cs": 0}

            def project(colq, colk, colv, gq, gk, rope):
                pidx = pstate["n"]
                pstate["n"] += 1
                assert PCOLS[pidx] == (colq, colk, colv)
                wg = wgs[pidx % 2]
                dst = ((qT, gq), (kT, gk))
                nT = len(TILES)

                def A(ti):
                    t0, w, isc = TILES[ti]
                    for j in range(2):
                        ps = PB[(ti % 3) * 2 + j]
                        for k in range(KC):
                            mm(ps[:, :w], wg[:, k, j * 128:(j + 1) * 128], hT[:, k, t0:t0 + w], k == 0, k == KC - 1,
                               [wg.r(j), hT.r(ti)], [ps.r()])

                def S_(ti):
                    t0, w, isc = TILES[ti]
                    for j in range(2):
                        ps = PB[(ti % 3) * 2 + j]
                        sq = tmp["sq2"][j]
                        pb = PB[6 + j]
                        act(sq[:, :w], ps[:, :w], AF.Square, [ps.r()], [sq.r()])
                        mm(pb[:, :w], blk_b[:], sq[:, :w], True, True, [blk_b.r(), sq.r()], [pb.r()])

                def F_(ti):
                    t0, w, isc = TILES[ti]
                    for j in range(2):
                        ps = PB[(ti % 3) * 2 + j]
                        pb = PB[6 + j]
                        dstt, gcol = dst[j]
                        out_ap, out_res = dstt[:, t0:t0 + w], [dstt.r(ti)]
                        act(tmp["rt"][:, :w], pb[:, :w], AF.Ln, [pb.r()], [tmp["rt"].r()], scale=1.0 / 64, bias=EPS)
                        act(tmp["R"][:, :w], tmp["rt"][:, :w], AF.Exp, [tmp["rt"].r()], [tmp["R"].r()], scale=-0.5)
                        if not (rope and not isc):
                            stt(out_ap, ps[:, :w], gcol, tmp["R"][:, :w], ALU.mult, ALU.mult, [ps.r(), prm.r(), tmp["R"].r()], out_res)
                            continue
                        qn = tmp["qn2"][j]
                        stt(qn[:, :w], ps[:, :w], gcol, tmp["R"][:, :w], ALU.mult, ALU.mult, [ps.r(), prm.r(), tmp["R"].r()], [qn.r()])
                        cs = tmp["cs"][state["cs"] % 2]
                        state["cs"] += 1
                        kb.dma("sp", cs[:, 0, :w], cd["cosT"][:, t0:t0 + w], writes=[cs.r()])
                        kb.dma("sp", cs[:, 1, :w], cd["sinT"][:, t0:t0 + w], writes=[cs.r()])
                        mm(pb[:, :w], rm_b[:], qn[:, :w], True, True, [rm_b.r(), qn.r()], [pb.r()])
                        tt("pool", tmp["t1"][:, :w], qn[:, :w], cs[:, 0, :w], ALU.mult, [qn.r(), cs.r()], [tmp["t1"].r()])
                        tt("dve", tmp["t2"][:, :w], pb[:, :w], cs[:, 1, :w], ALU.mult, [pb.r(), cs.r()], [tmp["t2"].r()])
                        tt("pool", out_ap, tmp["t1"][:, :w], tmp["t2"][:, :w], ALU.add, [tmp["t1"].r(), tmp["t2"].r()], out_res)

                A(0)
                A(1)
                for ti in range(nT):
                    S_(ti)
                    if ti + 2 < nT:
                        A(ti + 2)
                    F_(ti)
                for c in range(34):
                    ps = PB[4 + c % 2]
                    ti = min(c // 4, 8)
                    for k in range(KC):
                        mm(ps[:, 0:128], hT[:, k, c * 128:(c + 1) * 128], wg[:, k, 256:384], k == 0, k == KC - 1,
                           [hT.r(ti), wg.r(2)], [ps.r()])
                    cp("act" if c % 2 == 0 else "dve", vtm[:, c, :], ps[:, 0:128], [ps.r()], [vtm.r(c)])
                load_wg(pidx + 1)

            STB = [PB[0], PB[1], PB[2], PB[7]]

            def dense_attn2(q0, w, kchunks, out_ap, out_res):
                ob = state["ob"] % 2
                state["ob"] += 1
                o1, o2 = PB[3 + 2 * ob], PB[4 + 2 * ob]
                n = len(kchunks)

                def qk(i):
                    kc = kchunks[i]
                    for hl in range(2):
                        lo, hi = 64 * hl, 64 * hl + 64
                        st = STB[(2 * i + hl) % 4]
                        mm(st[:, :w], kT[lo:hi, kc * 128:(kc + 1) * 128], qT[lo:hi, q0:q0 + w], True, True,
                           [kT.r(min(kc // 4, 8)), qT.r(min(q0 // 512, 8))], [st.r()])

                def rest(i):
                    kc = kchunks[i]
                    for hl in range(2):
                        st = STB[(2 * i + hl) % 4]
                        e_ = E[(2 * i + hl) % 4]
                        act(e_[:, :w], st[:, :w], AF.Exp, [st.r()], [e_.r()])
                    for hl in range(2):
                        lo, hi = 64 * hl, 64 * hl + 64
                        e_ = E[(2 * i + hl) % 4]
                        mm(o1[lo:hi, :w], vtm[:, kc, lo:hi], e_[:, :w], i == 0, i == n - 1, [vtm.r(kc), e_.r()], [o1.r()])
                    for hl in range(2):
                        lo, hi = 64 * hl, 64 * hl + 64
                        e_ = E[(2 * i + hl) % 4]
                        mm(o2[lo:hi, :w], ones_b[:, 0:64], e_[:, :w], i == 0, i == n - 1, [ones_b.r(), e_.r()], [o2.r()])

                qk(0)
                for i in range(n):
                    if i + 1 < n:
                        qk(i + 1)
                    rest(i)
                rd = tmp["Rd"][0]
                act(rd[:, :w], o2[:, :w], AF.Ln, [o2.r()], [rd.r()])
                act(rd[:, :w], rd[:, :w], AF.Exp, [rd.r()], [rd.r()], scale=-1.0)
                tt("dve", out_ap, o1[:, :w], rd[:, :w], ALU.mult, [o1.r(), rd.r()], out_res)

            def na_attn2(chunk, Mts):
                cur = {}

                def geom(r):
                    w0 = min(max(r - 4, 0), 56)
                    par = w0 % 2
                    c0 = w0 // 2
                    nch = 4 + par
                    vi = NA_VARIANTS.index((par, w0 - r))
                    return nch, vi, [c0 + ch for ch in range(nch)] + [32, 33]

                def qk(r):
                    nch, vi, chunks = geom(r)
                    for ci, kc in enumerate(chunks):
                        for hl in range(2):
                            lo, hi = 64 * hl, 64 * hl + 64
                            st = STB[(2 * r + hl) % 4]
                            mm(st[:, ci * 64:(ci + 1) * 64], kT[lo:hi, kc * 128:(kc + 1) * 128], qT[lo:hi, r * 64:(r + 1) * 64], True, True,
                               [kT.r(min(kc // 4, 8)), qT.r(r // 8)], [st.r()])

                def rest(r):
                    r8, rr = divmod(r, 8)
                    if rr == 0:
                        cur["ob"] = state["ob"] % 2
                        state["ob"] += 1
                    ob = cur["ob"]
                    o1, o2 = PB[3 + 2 * ob], PB[4 + 2 * ob]
                    nch, vi, chunks = geom(r)
                    nk = len(chunks)
                    for hl in range(2):
                        st = STB[(2 * r + hl) % 4]
                        e_ = E[(2 * r + hl) % 4]
                        act(e_[:, :nk * 64], st[:, :nk * 64], AF.Exp, [st.r()], [e_.r()])
                        tt("dve", e_[:, :nch * 64], e_[:, :nch * 64], Mts[hl][:, vi, 0:nch, :].rearrange("p a b -> p (a b)"), ALU.mult,
                           [e_.r(), Mts[hl].r()], [e_.r()])
                    for ci, kc in enumerate(chunks):
                        for hl in range(2):
                            lo, hi = 64 * hl, 64 * hl + 64
                            e_ = E[(2 * r + hl) % 4]
                            mm(o1[lo:hi, rr * 64:(rr + 1) * 64], vtm[:, kc, lo:hi], e_[:, ci * 64:(ci + 1) * 64], ci == 0, ci == nk - 1,
                               [vtm.r(kc), e_.r()], [o1.r()])
                    for ci, kc in enumerate(chunks):
                        for hl in range(2):
                            lo, hi = 64 * hl, 64 * hl + 64
                            e_ = E[(2 * r + hl) % 4]
                            mm(o2[lo:hi, rr * 64:(rr + 1) * 64], ones_b[:, 0:64], e_[:, ci * 64:(ci + 1) * 64], ci == 0, ci == nk - 1,
                               [ones_b.r(), e_.r()], [o2.r()])
                    if rr == 7:
                        rd = tmp["Rd"][0]
                        act(rd[:, :], o2[:, :], AF.Ln, [o2.r()], [rd.r()])
                        act(rd[:, :], rd[:, :], AF.Exp, [rd.r()], [rd.r()], scale=-1.0)
                        tt("dve", ycatT[:, chunk, r8 * 512:(r8 + 1) * 512], o1[:, :], rd[:, :], ALU.mult, [o1.r(), rd.r()],
                           [ycatT.r((chunk, r8))])

                qk(0)
                for r in range(64):
                    if r + 1 < 64:
                        qk(r + 1)
                    rest(r)

            with kb.scope():
                tmp["qn2"] = [kb.sb("qn", [128, 512], BF16) for _ in range(2)]
                tmp["t1"] = kb.sb("t1", [128, 512], F32)
                tmp["t2"] = kb.sb("t2", [128, 512], F32)
                tmp["cs"] = [kb.sb("cs", [128, 2, 512], BF16) for _ in range(2)]
                for gp in range(2):
                    base = 3 * (DHY + 384)
                    mark("gaP%d" % gp)
                    project(base + gp * 128, base + 256 + gp * 128, base + 512 + gp * 128, prm[:, l, 65:66], prm[:, l, 55:56], True)
                    if stop == "proj":
                        dbg_dump("qT", qT[:], [128, NTOK], BF16)
                        dbg_dump("kT", kT[:], [128, NTOK], BF16)
                        dbg_dump("vtm", vtm[:], [128, 34, 128], BF16)
                        return
                    mark("gaA%d" % gp)
                    for qt in range(8):
                        dense_attn2(qt * 512, 512, list(range(34)), ycatT[:, 6 + gp, qt * 512:(qt + 1) * 512], [ycatT.r((6 + gp, qt))])
                    if l == 0:
                        dense_attn2(S, LC, [32, 33], ycatT[:, 6 + gp, S:NTOK], [ycatT.r((6 + gp, 8))])
            with kb.scope():
                ec2 = kb.sb("ec2", [128, 15, 64], BF16)
                Mts = [kb.sb("Mt", [128, len(NA_VARIANTS), 5, 64], BF16) for _ in range(2)]
                cmask = kb.sb("cmask", [128, 64], F32)
                stage = kb.sb("stage", [128, 15, 64], F32)
                kb.dma("sp", cmask[:], cd["colmask"][:, :], writes=[cmask.r()])
                for np_ in range(3):
                    base = 3 * DHY
                    mark("naP%d" % np_)
                    project(base + np_ * 128, base + 384 + np_ * 128, base + 768 + np_ * 128, prm[:, l, 64:65], prm[:, l, 53:54], False)
                    mark("naA%d" % np_)
                    for hl in range(2):
                        h = np_ * 2 + hl
                        Mt = Mts[hl]
                        for half in range(2):
                            kb.dma("sp", stage[half * 64:(half + 1) * 64], rpb_d.t[l, :, h * 15:(h + 1) * 15, :], writes=[stage.r()])
                        act(stage[:], stage[:], AF.Exp, [stage.r()], [stage.r()])
                        tt("dve", ec2[:], stage[:], cmask[:].unsqueeze(1).to_broadcast([128, 15, 64]), ALU.mult, [stage.r(), cmask.r()], [ec2.r()])
                        kb.op("pool", lambda e: e.memset(Mt[:], 0.0), [], [Mt.r()])
                        for vi, (par, dwr) in enumerate(NA_VARIANTS):
                            for krl in range(2):
                                chs = [ch for ch in range(5) if 0 <= 2 * ch + krl - par < 8]
                                lo_ch, n = chs[0], len(chs)
                                dr0 = 2 * lo_ch + krl - par + dwr + 7
                                cp("pool", Mt[64 * krl:64 * krl + 64, vi, lo_ch:lo_ch + n, :], ec2[64 * krl:64 * krl + 64, dr0:dr0 + 2 * n - 1:2, :],
                                   [ec2.r()], [Mt.r()])
                    na_attn2(3 + np_, Mts)
                    if l == 0:
                        dense_attn2(S, LC, [32, 33], ycatT[:, 3 + np_, S:NTOK], [ycatT.r((3 + np_, 8))])

    def hyena_proj(l, hT, ztm, ztc):
        src_w = win_d.t[l].rearrange("(k p) n -> p k n", p=128)
        RW = NTOK + 4
        with kb.scope():
            wg = kb.sb("wgh", [128, 8, 384], BF16)
            raw = [kb.sb("raw", [128, RW], BF16) for _ in range(2)]
            u1 = kb.sb("u1", [128, NTOK], BF16)
            zf = kb.sb("zf", [128, NTOK], BF16)
            x0 = kb.sb("x0", [128, NTOK], BF16)
            tc_ = [kb.sb("tc", [128, 1024], F32) for _ in range(2)]
            for rw in raw:
                for c in (0, S + 1, S + 2, RW - 1):
                    kb.op("pool", lambda e: e.memset(rw[:, c:c + 1], 0.0), [], [rw.r()])
            nraw = [0]
            pbf = PB[7].t[:, :].bitcast(BF16)

            def conv_chunk(cidx, j, mul_by=None, out=None, out_res=None):
                rw = raw[nraw[0] % 2]
                nraw[0] += 1
                for ti, (t0, w, isc) in enumerate(TILES):
                    ps = PB[ti % 4]
                    for k in range(KC):
                        mm(ps[:, :w], wg[:, k, j * 128:(j + 1) * 128], hT[:, k, t0:t0 + w], k == 0, k == KC - 1, [wg.r(j), hT.r(ti)], [ps.r()])
                    off = (S + 3) if isc else (1 + t0)
                    cp("act", rw[:, off:off + w], ps[:, :w], [ps.r()], [rw.r()])
                pc = 16 + cidx * 4
                segs = [(1 + a, a, 1024) for a in range(0, S, 1024)] + [(S + 3, S, LC)]
                for si, (ro, oo, n) in enumerate(segs):
                    t = tc_[si % 2]
                    ts("dve", t[:, :n], rw[:, ro:ro + n], prm[:, l, pc + 1:pc + 2], prm[:, l, pc + 3:pc + 4], ALU.mult, ALU.add, [rw.r(), prm.r()], [t.r()])
                    stt(t[:, :n], rw[:, ro - 1:ro - 1 + n], prm[:, l, pc:pc + 1], t[:, :n], ALU.mult, ALU.add, [rw.r(), prm.r(), t.r()], [t.r()])
                    if mul_by is None:
                        stt(out[:, oo:oo + n], rw[:, ro + 1:ro + 1 + n], prm[:, l, pc + 2:pc + 3], t[:, :n], ALU.mult, ALU.add, [rw.r(), prm.r(), t.r()], out_res)
                    else:
                        stt(t[:, :n], rw[:, ro + 1:ro + 1 + n], prm[:, l, pc + 2:pc + 3], t[:, :n], ALU.mult, ALU.add, [rw.r(), prm.r(), t.r()], [t.r()])
                        tt("pool", out[:, oo:oo + n], t[:, :n], mul_by[:, oo:oo + n], ALU.mult, [t.r(), mul_by.r()], out_res)

            for j in range(3):
                for jj, cidx in enumerate((j, 3 + j, 6 + j)):
                    kb.dma("pool", wg[:, :, jj * 128:(jj + 1) * 128], src_w[:, :, cidx * 128:(cidx + 1) * 128], writes=[wg.r(jj)])
                conv_chunk(3 + j, 1, out=u1, out_res=[u1.r()])
                conv_chunk(6 + j, 2, mul_by=u1, out=zf, out_res=[zf.r()])
                conv_chunk(j, 0, out=x0, out_res=[x0.r()])
                kb.dma("sp", x0c_d.t[:, j, :], x0[:], reads=[x0.r()], writes=[x0c_d.r()])
                for g in range(5):
                    nt = 8 if g < 4 else 2
                    for i in range(nt):
                        tcn = g * 8 + i
                        kb.op("pe", lambda e: e.transpose(pbf[:, i * 128:(i + 1) * 128], zf[:, tcn * 128:(tcn + 1) * 128], ident_b[:]),
                              [zf.r(), ident_b.r()], [PB[7].r()])
                    if g < 4:
                        cp("act", ztm[:, g * 8:(g + 1) * 8, j * 128:(j + 1) * 128], pbf[:, :].rearrange("p (a b) -> p a b", b=128), [PB[7].r()], [ztm.r()])
                    else:
                        cp("act", ztc[:, :, j * 128:(j + 1) * 128], pbf[:, 0:256].rearrange("p (a b) -> p a b", b=128), [PB[7].r()], [ztc.r()])

    def hyena_filter(l, L, hsum, hdiff):
        NT = L // 128
        W = min(512, L)
        with kb.scope():
            h3 = kb.sb("h3", [64, L], F32)
            zp = [kb.sb("zp", [33, 512], F32) for _ in range(2)]
            w1 = kb.sb("w1", [33, 64], F32)
            w2 = kb.sb("w2", [64, 2, 64], F32)
            w3 = kb.sb("w3", [64, 768], F32)
            hv = kb.sb("hv", [64, 8], F32)
            negt = kb.sb("negt", [128, NT], F32)
            dabs = kb.sb("dabs", [128, 384], F32)
            skip = kb.sb("skip", [1, 384], F32)
            arg = [kb.sb("arg", [64, 512], F32) for _ in range(2)]
            mk = [kb.sb("mk", [64, 512], F32) for _ in range(2)]
            hh = [kb.sb("hh", [64, 512], F32) for _ in range(2)]
            dec = [kb.sb("dec", [128, 384], F32) for _ in range(2)]
            hf = [kb.sb("hf", [128, 384], F32) for _ in range(2)]
            hb = [kb.sb("hb", [128, 384], F32) for _ in range(2)]
            kb.dma("sp", w1[:], hyw1_d.t[l], writes=[w1.r()])
            kb.dma("sp", w2[:, 0, :], hyw2_d.t[l, 0], writes=[w2.r()])
            kb.dma("sp", w2[:, 1, :], hyw2_d.t[l, 1], writes=[w2.r()])
            kb.dma("sp", w3[:], hyw3_d.t[l], writes=[w3.r()])
            kb.dma("sp", hv[:, 0:4], hyv_d.t[l], writes=[hv.r()])
            kb.dma("sp", negt[:], cd["negt%d" % L][:, :], writes=[negt.r()])
            kb.dma("sp", dabs[:], cd["dabs"][:, :], writes=[dabs.r()])
            kb.dma("sp", skip[:], hysk_d.t[l], writes=[skip.r()])
            ts("dve", hv[:, 4:7], hv[:, 1:4], hv[:, 0:1], None, ALU.mult, None, [hv.r()], [hv.r()])
            cnt = [0]

            def sinact(ps, w, fbcol, out_ap, out_res):
                i = cnt[0] % 2
                cnt[0] += 1
                a, m = arg[i], mk[i]
                ts("dve", a[:, :w], ps[0:64, :w], hv[:, 0:1], hv[:, fbcol:fbcol + 1], ALU.mult, ALU.add, [ps.r(), hv.r()], [a.r()])
                ts("dve", m[:, :w], a[:, :w], PI, -2.0 * PI, ALU.is_gt, ALU.mult, [a.r()], [m.r()])
                tt("pool", a[:, :w], a[:, :w], m[:, :w], ALU.add, [a.r(), m.r()], [a.r()])
                ts("dve", m[:, :w], a[:, :w], -PI, 2.0 * PI, ALU.is_lt, ALU.mult, [a.r()], [m.r()])
                tt("pool", a[:, :w], a[:, :w], m[:, :w], ALU.add, [a.r(), m.r()], [a.r()])
                act(out_ap, a[:, :w], AF.Sin, [a.r()], out_res)

            for tix in range(L // W):
                z = zp[tix % 2]
                kb.dma("sp", z[:, :W], cd["zpos%d" % L][:, tix * W:(tix + 1) * W], writes=[z.r()])
                ps = PB[tix % 2]
                mm(ps[0:64, :W], w1[:], z[:, :W], True, True, [w1.r(), z.r()], [ps.r()])
                h_ = hh[0]
                sinact(ps, W, 4, h_[:, :W], [h_.r()])
                ps2 = PB[2 + tix % 2]
                mm(ps2[0:64, :W], w2[:, 0, :], h_[:, :W], True, True, [w2.r(), h_.r()], [ps2.r()])
                h2_ = hh[1]
                sinact(ps2, W, 5, h2_[:, :W], [h2_.r()])
                ps3 = PB[4 + tix % 2]
                mm(ps3[0:64, :W], w2[:, 1, :], h2_[:, :W], True, True, [w2.r(), h2_.r()], [ps3.r()])
                sinact(ps3, W, 6, h3[:, tix * W:(tix + 1) * W], [h3.r()])
            for tc in range(NT):
                i = tc % 2
                pf, pb_ = PB[i * 2], PB[i * 2 + 1]
                mm(pf[:, 0:384], h3[:, tc * 128:(tc + 1) * 128], w3[:, 0:384], True, True, [h3.r(), w3.r()], [pf.r()])
                mm(pb_[:, 0:384], h3[:, tc * 128:(tc + 1) * 128], w3[:, 384:768], True, True, [h3.r(), w3.r()], [pb_.r()])
                act(dec[i][:], dabs[:], AF.Exp, [dabs.r(), negt.r()], [dec[i].r()], scale=negt[:, tc:tc + 1])
                tt("dve", hf[i][:], pf[:, 0:384], dec[i][:], ALU.mult, [pf.r(), dec[i].r()], [hf[i].r()])
                tt("dve", hb[i][:], pb_[:, 0:384], dec[i][:], ALU.mult, [pb_.r(), dec[i].r()], [hb[i].r()])
                if tc == 0:
                    kb.op("dve", lambda e: e.memset(hb[i][0:1, :], 0.0), [], [hb[i].r()])
                    tt("dve", hf[i][0:1, :], hf[i][0:1, :], skip[:], ALU.add, [hf[i].r(), skip.r()], [hf[i].r()])
                tt("pool", hsum[:, tc, :], hf[i][:], hb[i][:], ALU.add, [hf[i].r(), hb[i].r()], [hsum.r()])
                tt("pool", hdiff[:, tc, :], hb[i][:], hf[i][:], ALU.subtract, [hf[i].r(), hb[i].r()], [hdiff.r()])
            dbg_dump("hsum%d_%d" % (l, L), hsum[:], [128, NT, 384], BF16)

    def hyena_dft(l, L, ztm, ycatT, col0):
        NT = L // 128
        W = min(512, L)
        NP = max(1, NT // 8)
        FP = NT // NP
        NH = 2 if NT >= 16 else 1
        HT = NT // NH
        with kb.scope():
            hsum = kb.sb("hsum", [128, NT, 384], BF16)
            hdiff = kb.sb("hdiff", [128, NT, 384], BF16)
            hyena_filter(l, L, hsum, hdiff)
            pq = kb.sb("pq", [128, NT, 2, 384], BF16)
            with kb.scope():
                cbuf = [kb.sb("cbuf", [128, HT, 128], BF16) for _ in range(3)]
                sbuf = [kb.sb("sbuf", [128, HT, 128], BF16) for _ in range(3)]
                t1 = [kb.sb("t1h", [128, 384], F32) for _ in range(2)]
                t2 = [kb.sb("t2h", [128, 384], F32) for _ in range(2)]
                nb = 0
                for ps_ in range(2):
                    mc, ms = (hsum, hdiff) if ps_ == 0 else (ztm, ztm)
                    for fc in range(NT):
                        ba, bb = PB[(fc % 2) * 2], PB[(fc % 2) * 2 + 1]
                        for hh_ in range(NH):
                            cb, sb_ = cbuf[nb % 3], sbuf[nb % 3]
                            nb += 1
                            kb.dma("sp", cb[:], cd["FWC%d" % L][fc, :, hh_ * HT:(hh_ + 1) * HT, :], writes=[cb.r()])
                            kb.dma("sp", sb_[:], cd["FWS%d" % L][fc, :, hh_ * HT:(hh_ + 1) * HT, :], writes=[sb_.r()])
                            for t_ in range(HT):
                                tc = hh_ * HT + t_
                                mm(ba[:, 0:384], cb[:, t_, :], mc[:, tc, :], tc == 0, tc == NT - 1, [cb.r(), mc.r()], [ba.r()])
                                mm(bb[:, 0:384], sb_[:, t_, :], ms[:, tc, :], tc == 0, tc == NT - 1, [sb_.r(), ms.r()], [bb.r()])
                        if ps_ == 0:
                            cp("act", pq[:, fc, 0, :], ba[:, 0:384], [ba.r()], [pq.r(fc)])
                            cp("dve", pq[:, fc, 1, :], bb[:, 0:384], [bb.r()], [pq.r(fc)])
                        else:
                            i = fc % 2
                            tt("dve", t1[i][:], ba[:, 0:384], pq[:, fc, 0, :], ALU.mult, [ba.r(), pq.r(fc)], [t1[i].r()])
                            tt("dve", t2[i][:], bb[:, 0:384], pq[:, fc, 1, :], ALU.mult, [bb.r(), pq.r(fc)], [t2[i].r()])
                            tt("pool", t1[i][:], t1[i][:], t2[i][:], ALU.add, [t1[i].r(), t2[i].r()], [t1[i].r()])
                            tt("dve", t2[i][:], bb[:, 0:384], pq[:, fc, 0, :], ALU.mult, [bb.r(), pq.r(fc)], [t2[i].r()])
                            cp("pool", pq[:, fc, 0, :], t1[i][:], [t1[i].r()], [pq.r(fc)])
                            tt("dve", t1[i][:], ba[:, 0:384], pq[:, fc, 1, :], ALU.mult, [ba.r(), pq.r(fc)], [t1[i].r()])
                            tt("pool", pq[:, fc, 1, :], t2[i][:], t1[i][:], ALU.subtract, [t1[i].r(), t2[i].r()], [pq.r(fc)])
                    kb.barrier()
            with kb.scope():
                ivc = [kb.sb("ivc", [128, FP, W], BF16) for _ in range(2)]
                ivs = [kb.sb("ivs", [128, FP, W], BF16) for _ in range(2)]
                x0t = [kb.sb("x0t", [128, 3, W], BF16) for _ in range(2)]
                nb = 0
                for tti in range(L // W):
                    accs = [PB[(tti % 2) * 3 + cc] for cc in range(3)]
                    xt_ = x0t[tti % 2]
                    c0 = col0 + tti * W
                    kb.dma("sp", xt_[:], x0c_d.t[:, :, c0:c0 + W], reads=[x0c_d.r()], writes=[xt_.r()])
                    for part in range(NP):
                        ic, is_ = ivc[nb % 2], ivs[nb % 2]
                        nb += 1
                        kb.dma("sp", ic[:], cd["IVC%d" % L][tti, :, part * FP:(part + 1) * FP, :], writes=[ic.r()])
                        kb.dma("sp", is_[:], cd["IVS%d" % L][tti, :, part * FP:(part + 1) * FP, :], writes=[is_.r()])
                        for fi in range(FP):
                            fc = part * FP + fi
                            for cc in range(3):
                                first = (part == 0 and fi == 0)
                                last = (part == NP - 1 and fi == FP - 1)
                                mm(accs[cc][:, :W], pq[:, fc, 0, cc * 128:(cc + 1) * 128], ic[:, fi, :], first, False, [pq.r(fc), ic.r()], [accs[cc].r()])
                                mm(accs[cc][:, :W], pq[:, fc, 1, cc * 128:(cc + 1) * 128], is_[:, fi, :], False, last, [pq.r(fc), is_.r()], [accs[cc].r()])
                    for cc in range(3):
                        stt(ycatT[:, cc, c0:c0 + W], accs[cc][:, :W], 1.0 / L, xt_[:, cc, :], ALU.mult, ALU.mult, [accs[cc].r(), xt_.r()],
                            [ycatT.r((cc, c0 // 512))])

    def group_norm(l, ycatT, ntiles):
        with kb.scope():
            tmp = {"sq": kb.sb("sqg", [128, 2, 512], BF16), "rt": kb.sb("rtg", [128, 512], F32), "R": kb.sb("Rg", [128, 512], F32)}
            for ti in range(ntiles):
                t0, w, isc = TILES[ti]
                for gi, (ks, n) in enumerate((((0, 1, 2), 384), ((3, 4, 5), 384), ((6, 7), 256))):
                    pb = PB[(ti * 3 + gi) % 4]
                    rms_tile(w, n, [(ycatT[:, k, t0:t0 + w], [ycatT.r((k, ti))]) for k in ks], pb, tmp)
                    for k in ks:
                        stt(ycatT[:, k, t0:t0 + w], ycatT[:, k, t0:t0 + w], prm[:, l, 56 + k:57 + k], tmp["R"][:, :w], ALU.mult, ALU.mult,
                            [ycatT.r((k, ti)), prm.r(), tmp["R"].r()], [ycatT.r((k, ti))])

    def router_tile(ti, h32):
        maskall, comball = rt["mask"], rt["comb"]
        for c4 in range(4):
            ch = ti * 4 + c4
            ps = PB[4]
            for k in range(KC):
                mm(ps[:, 0:8], h32[:, k, c4 * 128:(c4 + 1) * 128], wr32[:, k, :], k == 0, k == KC - 1, [h32.r(), wr32.r()], [ps.r()])
            cp("dve", lg[:, 0:8], ps[:, 0:8], [ps.r()], [lg.r()])
            kb.op("dve", lambda e: e.max(out=lg[:, 8:16], in_=lg[:, 0:8]), [lg.r()], [lg.r()])
            ts("dve", maskall[:, ch, :], lg[:, 0:8], lg[:, 9:10], None, ALU.is_ge, None, [lg.r()], [maskall.r()])
            ts("dve", lg[:, 24:25], lg[:, 8:9], -1.0, None, ALU.mult, None, [lg.r()], [lg.r()])
            act(lg[:, 32:40], lg[:, 0:8], AF.Exp, [lg.r()], [lg.r()], bias=lg[:, 24:25])
            tt("dve", lg[:, 32:40], lg[:, 32:40], maskall[:, ch, :], ALU.mult, [lg.r(), maskall.r()], [lg.r()])
            kb.op("dve", lambda e: e.tensor_reduce(out=lg[:, 25:26], in_=lg[:, 32:40], axis=AX.X, op=ALU.add), [lg.r()], [lg.r()])
            recip(lg[:, 26:27], lg[:, 25:26], [lg.r()], [lg.r()])
            ts("dve", comball[:, ch, :], lg[:, 32:40], lg[:, 26:27], None, ALU.mult, None, [lg.r()], [comball.r()])

    def wout_phase(l, ycatT, ntiles, moe):
        with kb.scope():
            wo = kb.sb("wo", [128, 8, D], BF16)
            kb.dma("pool", wo[:], wout_d.t[l].rearrange("(k p) n -> p k n", p=128), writes=[wo.r()])
            tmp = norm_tmp()
            xo = [kb.sb("xo", [128, 8, 512], F32) for _ in range(2)]
            xn = [kb.sb("xn", [128, 8, 512], F32) for _ in range(2)]
            h2t = [kb.sb("h2t", [128, 8, 512], BF16) for _ in range(1 if moe else 2)]
            h32 = kb.sb("h32", [128, 8, 512], F32) if moe else None
            htm = [kb.sb("htm", [128, D], BF16) for _ in range(2)] if moe else None
            xtm = [kb.sb("xtm", [128, D], F32) for _ in range(2)] if moe else None
            pbf6 = PB[6].t[:, :].bitcast(BF16)
            if moe:
                rt["mask"] = kb.sb("maskall", [128, 32, 8], BF16)
                rt["comb"] = kb.sb("comball", [128, 32, 8], F32)
            def W_(ti):
                t0, w, isc = TILES[ti]
                wh = 1 if isc else 0
                xo_, xn_ = xo[ti % 2], xn[ti % 2]
                if ti == 0:
                    kb.dma("sp", xo_[:, :, :w], xTv[:, :, t0:t0 + w], reads=[xT_d.r(ti)], writes=[xo_.r()])
                if ti + 1 < ntiles:
                    t1_, w1_, _ = TILES[ti + 1]
                    kb.dma("sp", xo[(ti + 1) % 2][:, :, :w1_], xTv[:, :, t1_:t1_ + w1_], reads=[xT_d.r(ti + 1)], writes=[xo[(ti + 1) % 2].r()])
                for oc in range(KC):
                    ps = PB[oc % 4]
                    for k in range(KC):
                        mm(ps[:, :w], wo[:, k, oc * 128:(oc + 1) * 128], ycatT[:, k, t0:t0 + w], k == 0, k == KC - 1,
                           [wo.r(), ycatT.r((k, ti))], [ps.r()])
                    stt(xn_[:, oc, :w], ps[:, :w], aff[:, 2, oc, wh:wh + 1], xo_[:, oc, :w], ALU.mult, ALU.add, [ps.r(), aff.r(), xo_.r()], [xn_.r()])
                if not moe:
                    kb.dma("sp", xTv[:, :, t0:t0 + w], xn_[:, :, :w], reads=[xn_.r()], writes=[xT_d.r(ti)])

            def N_(ti):
                t0, w, isc = TILES[ti]
                xn_, h2_ = xn[ti % 2], h2t[ti % len(h2t)]
                norm_tile(l, 1, xn_, ti, lambda k: (h2_[:, k, :w], [h2_.r()]), tmp, h32=h32)
                if not moe:
                    kb.dma("sp", h2_d.t[:, :, t0:t0 + w], h2_[:, :, :w], reads=[h2_.r()], writes=[h2_d.r(ti)])
                    return
                router_tile(ti, h32)
                for c4 in range(4):
                    r0 = t0 + c4 * 128
                    hb, xb = htm[c4 % 2], xtm[c4 % 2]
                    for k in range(KC):
                        kb.op("pe", lambda e: e.transpose(pbf6[:, k * 128:(k + 1) * 128], h2_[:, k, c4 * 128:(c4 + 1) * 128], ident_b[:]),
                              [h2_.r(), ident_b.r()], [PB[6].r()])
                    cp("act", hb[:], pbf6[:, :], [PB[6].r()], [hb.r()])
                    kb.dma("sp", h2tm_d.t[r0:r0 + 128, :], hb[:], reads=[hb.r()], writes=[h2tm_d.r(ti * 4 + c4)])
                    for kh in range(2):
                        pb = PB[5]
                        for kk in range(4):
                            k = kh * 4 + kk
                            kb.op("pe", lambda e: e.transpose(pb[:, kk * 128:(kk + 1) * 128], xn_[:, k, c4 * 128:(c4 + 1) * 128], ident[:]),
                                  [xn_.r(), ident.r()], [pb.r()])
                        cp("dve" if kh == 0 else "act", xb[:, kh * 512:(kh + 1) * 512], pb[:, :], [pb.r()], [xb.r()])
                    kb.dma("sp", xtm_d.t[r0:r0 + 128, :], xb[:], reads=[xb.r()], writes=[xtm_d.r(ti * 4 + c4)])

            W_(0)
            for ti in range(ntiles):
                if ti + 1 < ntiles:
                    W_(ti + 1)
                N_(ti)
            if moe:
                kb.dma("sp", mask_d.t[:, :], rt["mask"][:].rearrange("p c e -> p (c e)"), reads=[rt["mask"].r()], writes=[mask_d.r()])
                kb.dma("sp", comb_d.t[:, :], rt["comb"][:].rearrange("p c e -> p (c e)"), reads=[rt["comb"].r()], writes=[comb_d.r()])

    def ffn_phase(l, ntiles, passes):
        with kb.scope():
            h2T = kb.sb("h2T", [128, 8, NTOK], BF16)
            for ti in range(ntiles):
                t0, w, isc = TILES[ti]
                kb.dma("sp", h2T[:, :, t0:t0 + w], h2_d.t[:, :, t0:t0 + w], reads=[h2_d.r(ti)], writes=[h2T.r(ti)])
            wbufs = [(kb.sb("wgt", [128, 8, 896], BF16), kb.sb("wut", [128, 8, 896], BF16), kb.sb("wdt", [128, 7, D], BF16)) for _ in range(2)]
            NXO = 6
            xo = [kb.sb("fxo", [128, 512], F32) for _ in range(NXO)]
            xn = [kb.sb("fxn", [128, 512], F32) for _ in range(NXO)]
            a_b = [kb.sb("fa", [128, 7, 512], BF16) for _ in range(2)]
            sgb = [kb.sb("fsg", [128, 512], BF16) for _ in range(2)]

            def load_w(pi):
                wg_src, wu_src, wd_src, ff0, nff, ex = passes[pi]
                wgt, wut, wdt = wbufs[pi % 2]
                kb.dma("pool", wgt[:, :, :nff * 128], wg_src[:, :, ff0 * 128:(ff0 + nff) * 128], writes=[wgt.r()])
                kb.dma("pool", wut[:, :, :nff * 128], wu_src[:, :, ff0 * 128:(ff0 + nff) * 128], writes=[wut.r()])
                kb.dma("pool", wdt[:, :nff, :], wd_src[ff0 * 128:(ff0 + nff) * 128, :].rearrange("(j p) n -> p j n", p=128), writes=[wdt.r()])

            load_w(0)
            items = [(pi, ti, oc) for pi in range(len(passes)) for ti in range(ntiles) for oc in range(KC)]
            PF = 3
            nload = [0]

            def ensure_loaded(upto):
                while nload[0] < min(upto, len(items)):
                    n = nload[0]
                    _, ti_, oc_ = items[n]
                    t0_, w_, _ = TILES[ti_]
                    kb.dma("sp", xo[n % NXO][:, :w_], xTv[:, oc_, t0_:t0_ + w_], reads=[xT_d.r((ti_, oc_))], writes=[xo[n % NXO].r()])
                    nload[0] += 1

            cnt = 0
            for pi, (wg_src, wu_src, wd_src, ff0, nff, expert) in enumerate(passes):
                final = False
                wgt, wut, wdt = wbufs[pi % 2]
                for ti in range(ntiles):
                    if ti == 1 and pi + 1 < len(passes):
                        load_w(pi + 1)
                    t0, w, isc = TILES[ti]
                    wh = 1 if isc else 0
                    a_ = a_b[(pi * ntiles + ti) % 2]
                    for j in range(nff):
                        pg, pu = PB[(j % 2) * 2], PB[(j % 2) * 2 + 1]
                        for k in range(KC):
                            mm(pg[:, :w], wgt[:, k, j * 128:(j + 1) * 128], h2T[:, k, t0:t0 + w], k == 0, k == KC - 1, [wgt.r(), h2T.r(ti)], [pg.r()])
                        for k in range(KC):
                            mm(pu[:, :w], wut[:, k, j * 128:(j + 1) * 128], h2T[:, k, t0:t0 + w], k == 0, k == KC - 1, [wut.r(), h2T.r(ti)], [pu.r()])
                        sg = sgb[j % 2]
                        act(sg[:, :w], pg[:, :w], AF.Silu, [pg.r()], [sg.r()])
                        tt("dve", a_[:, j, :w], pu[:, :w], sg[:, :w], ALU.mult, [pu.r(), sg.r()], [a_.r(j)])
                    for oc in range(KC):
                        ensure_loaded(cnt + 1 + PF)
                        xo_, xn_ = xo[cnt % NXO], xn[cnt % NXO]
                        cnt += 1
                        ps = PB[4 + oc % 2]
                        for j in range(nff):
                            mm(ps[:, :w], wdt[:, j, oc * 128:(oc + 1) * 128], a_[:, j, :w], j == 0, j == nff - 1, [wdt.r(), a_.r(j)], [ps.r()])
                        stt(xn_[:, :w], ps[:, :w], aff[:, 5, oc, wh:wh + 1], xo_[:, :w], ALU.mult, ALU.add, [ps.r(), aff.r(), xo_.r()], [xn_.r()])
                        kb.dma("sp", xTv[:, oc, t0:t0 + w], xn_[:, :w], reads=[xn_.r()], writes=[xT_d.r((ti, oc))])

    def moe_sparse_phase():
        mg_rows = mg_d.t[0].rearrange("e r (q n) -> (e r q) n", n=896)
        mu_rows = mu_d.t[0].rearrange("e r (q n) -> (e r q) n", n=896)
        md_rows = md_d.t[0].rearrange("e r n -> (e r) n")
        IOA = bass.IndirectOffsetOnAxis
        with kb.scope():
            idx_hi = kb.sb("idx_hi", [128, 32], I32)
            idx_lo = kb.sb("idx_lo", [128, 32], I32)
            chi = kb.sb("chi", [128, 32], F32)
            clo = kb.sb("clo", [128, 32], F32)
            igu = kb.sb("igu", [128, NTM, 32], I32)
            idn = kb.sb("idn", [128, NTM, 28], I32)
            g5b = kb.sb("g5b", [128, D], F32)
            with kb.scope():
                g5r = kb.sb("g5r", [8, 128], F32)
                g5c = kb.sb("g5c", [128, 8], F32)
                cp("dve", g5c[:], modT[:, 40:48, 0], [modT.r()], [g5c.r()])
                kb.op("pe", lambda e: e.transpose(PB[0][0:8, 0:128], g5c[:], ident[:]), [g5c.r(), ident.r()], [PB[0].r()])
                cp("dve", g5r[:], PB[0][0:8, 0:128], [PB[0].r()], [g5r.r()])
                kb.dma("sp", g5_d.t.rearrange("o (k p) -> (o k) p", p=128), g5r[:], reads=[g5r.r()], writes=[g5_d.r()])
                kb.dma("sp", g5b[:], g5_d.t[0:1, :].to_broadcast([128, D]), reads=[g5_d.r()], writes=[g5b.r()])
            with kb.scope():
                ut_b = kb.sb("ut_b", [128, 128], BF16)
                thr = kb.sb("thr", [128, 8, 8], F32)
                tidx = kb.sb("tidx", [128, NTM, 8], F32)
                cgu = kb.sb("cgu", [128, 32], F32)
                cdn = kb.sb("cdn", [128, 28], F32)
                kb.dma("sp", ut_b[:], cd["ut_b"][:, :], writes=[ut_b.r()])
                kb.dma("sp", thr[:].rearrange("p a b -> p (a b)"), cd["thr"][:, :], writes=[thr.r()])
                kb.dma("sp", tidx[:].rearrange("p a b -> p (a b)"), cd["tidx"][:, :], writes=[tidx.r()])
                kb.dma("sp", cgu[:], cd["cgu"][:, :], writes=[cgu.r()])
                kb.dma("sp", cdn[:], cd["cdn"][:, :], writes=[cdn.r()])
                maskall = kb.sb("maskall2", [128, 32, 8], BF16)
                comball = kb.sb("comball2", [128, 32, 8], F32)
                kb.dma("sp", maskall[:].rearrange("p c e -> p (c e)"), mask_d.t[:, :], reads=[mask_d.r()], writes=[maskall.r()])
                kb.dma("sp", comball[:].rearrange("p c e -> p (c e)"), comb_d.t[:, :], reads=[comb_d.r()], writes=[comball.r()])
                mask_b = maskall
                cnt = kb.sb("cnt", [128, 32, 8], F32)
                cum = kb.sb("cum", [128, 33, 8], F32)
                pos = kb.sb("pos", [128, 32, 8], F32)
                cmpt = kb.sb("cmpt", [128, 8, 8], F32)
                ntile = kb.sb("ntile", [128, 8], F32)
                cumt = kb.sb("cumt", [128, 8], F32)
                base = kb.sb("base", [128, 8], F32)
                vals = kb.sb("vals", [128, 32, 8], F32)
                vals2 = kb.sb("vals2", [128, 32, 8], F32)
                eq = kb.sb("eq", [128, 32, 8], F32)
                hi = kb.sb("hi", [128, 32], F32)
                lo2 = kb.sb("lo2", [128, 32], F32)
                tf = kb.sb("tf", [128, 32], F32)
                cmp2 = kb.sb("cmp2", [128, NTM, 8], F32)
                etf = kb.sb("etf", [128, NTM], F32)
                fgu = kb.sb("fgu", [128, NTM, 32], F32)
                fdn = kb.sb("fdn", [128, NTM, 28], F32)

                def red(out, in_, op, rd, wr):
                    kb.op("dve", lambda e: e.tensor_reduce(out=out, in_=in_, axis=AX.X, op=op), rd, wr)

                bc, bp = PB[0], PB[1]
                mm(bc[:, 0:256], ones_b[:], mask_b[:].rearrange("p c e -> p (c e)"), True, True, [ones_b.r(), mask_b.r()], [bc.r()])
                for c in range(32):
                    mm(bp[:, c * 8:(c + 1) * 8], ut_b[:], mask_b[:, c, :], True, True, [ut_b.r(), mask_b.r()], [bp.r()])
                cp("dve", cnt[:].rearrange("p c e -> p (c e)"), bc[:, 0:256], [bc.r()], [cnt.r()])
                kb.op("dve", lambda e: e.memset(cum[:, 0, :], 0.0), [], [cum.r()])
                for c in range(32):
                    tt("dve", cum[:, c + 1, :], cum[:, c, :], cnt[:, c, :], ALU.add, [cum.r(), cnt.r()], [cum.r()])
                tt("dve", pos[:].rearrange("p c e -> p (c e)"), bp[:, 0:256], cum[:, 0:32, :].rearrange("p c e -> p (c e)"), ALU.add,
                   [bp.r(), cum.r()], [pos.r()])
                tt("dve", cmpt[:], thr[:], cum[:, 32, :].unsqueeze(2).to_broadcast([128, 8, 8]), ALU.is_lt, [thr.r(), cum.r()], [cmpt.r()])
                red(ntile[:], cmpt[:], ALU.add, [cmpt.r()], [ntile.r()])
                cp("dve", cumt[:, 0:1], ntile[:, 0:1], [ntile.r()], [cumt.r()])
                for e_ in range(1, 8):
                    tt("dve", cumt[:, e_:e_ + 1], cumt[:, e_ - 1:e_], ntile[:, e_:e_ + 1], ALU.add, [cumt.r(), ntile.r()], [cumt.r()])
                tt("dve", base[:], cumt[:], ntile[:], ALU.subtract, [cumt.r(), ntile.r()], [base.r()])
                ts("dve", base[:], base[:], 512.0, None, ALU.mult, None, [base.r()], [base.r()])
                tt("dve", vals[:], pos[:], base[:].unsqueeze(1).to_broadcast([128, 32, 8]), ALU.add, [pos.r(), base.r()], [vals.r()])
                stt(vals[:], vals[:], 1.0, maskall[:], ALU.add, ALU.mult, [vals.r(), maskall.r()], [vals.r()])
                red(hi[:], vals[:], ALU.max, [vals.r()], [hi.r()])
                stt(vals2[:], maskall[:], BIG, vals[:], ALU.mult, ALU.subtract, [vals.r(), maskall.r()], [vals2.r()])
                red(lo2[:], vals2[:], ALU.max, [vals2.r()], [lo2.r()])
                ts("dve", tf[:], hi[:], -1.0, None, ALU.add, None, [hi.r()], [tf.r()])
                cp("dve", idx_hi[:], tf[:], [tf.r()], [idx_hi.r()])
                ts("dve", tf[:], lo2[:], -1.0, BIG - 1.0, ALU.mult, ALU.add, [lo2.r()], [tf.r()])
                cp("dve", idx_lo[:], tf[:], [tf.r()], [idx_lo.r()])
                tt("dve", eq[:], vals[:], hi[:].unsqueeze(2).to_broadcast([128, 32, 8]), ALU.is_equal, [vals.r(), hi.r()], [eq.r()])
                tt("dve", eq[:], eq[:], comball[:], ALU.mult, [eq.r(), comball.r()], [eq.r()])
                red(chi[:], eq[:], ALU.add, [eq.r()], [chi.r()])
                tt("dve", eq[:], vals2[:], lo2[:].unsqueeze(2).to_broadcast([128, 32, 8]), ALU.is_equal, [vals2.r(), lo2.r()], [eq.r()])
                tt("dve", eq[:], eq[:], comball[:], ALU.mult, [eq.r(), comball.r()], [eq.r()])
                red(clo[:], eq[:], ALU.add, [eq.r()], [clo.r()])
                tt("dve", cmp2[:], tidx[:], cumt[:].unsqueeze(1).to_broadcast([128, NTM, 8]), ALU.is_ge, [tidx.r(), cumt.r()], [cmp2.r()])
                red(etf[:], cmp2[:], ALU.add, [cmp2.r()], [etf.r()])
                ts("dve", etf[:], etf[:], 7.0, None, ALU.min, None, [etf.r()], [etf.r()])
                stt(fgu[:], etf[:].unsqueeze(2).to_broadcast([128, NTM, 32]), 4096.0, cgu[:].unsqueeze(1).to_broadcast([128, NTM, 32]),
                    ALU.mult, ALU.add, [etf.r(), cgu.r()], [fgu.r()])
                cp("dve", igu[:], fgu[:], [fgu.r()], [igu.r()])
                stt(fdn[:], etf[:].unsqueeze(2).to_broadcast([128, NTM, 28]), 3584.0, cdn[:].unsqueeze(1).to_broadcast([128, NTM, 28]),
                    ALU.mult, ALU.add, [etf.r(), cdn.r()], [fdn.r()])
                cp("dve", idn[:], fdn[:], [fdn.r()], [idn.r()])
                dbg_dump("r_idx_hi", idx_hi[:], [128, 32], I32)
                dbg_dump("r_idx_lo", idx_lo[:], [128, 32], I32)
                dbg_dump("r_chi", chi[:], [128, 32], F32)
                dbg_dump("r_clo", clo[:], [128, 32], F32)
                dbg_dump("r_igu", igu[:], [128, NTM, 32], I32)
                dbg_dump("r_mask", maskall[:], [128, 32, 8], BF16)
            with kb.scope():
                wgu = [(kb.sb("wgt", [128, 8, 896], BF16), kb.sb("wut", [128, 8, 896], BF16)) for _ in range(2)]
                wd = kb.sb("wdt", [128, 28, D], BF16)
                a_ = kb.sb("fa", [128, 28, 512], BF16)
                hTt = [kb.sb("hTt", [128, 8, 512], BF16) for _ in range(2)]
                hsb = [kb.sb("hsb", [128, D], BF16) for _ in range(4)]
                sgb = [kb.sb("fsg", [128, 512], BF16) for _ in range(2)]
                ytb = [kb.sb("yt", [128, D], F32) for _ in range(2)]
                pbf6 = PB[6].t[:, :].bitcast(BF16)
                sc_res = []
                for c in range(32):
                    hs = hsb[c % 4]
                    kb.dma("sp", hs[:], h2tm_d.t[c * 128:(c + 1) * 128, :], reads=[h2tm_d.r(c)], writes=[hs.r()])
                    for nm, ix in (("h", idx_hi), ("l", idx_lo)):
                        kb.idma(hslot_d.t[:, :], IOA(ap=ix[:, c:c + 1], axis=0), hs[:], None, reads=[hs.r(), ix.r(), hslot_d.r()],
                                writes=[hslot_d.r((nm, c))])
                        sc_res.append(hslot_d.r((nm, c)))

                def load_wgu(i, q):
                    wgt, wut = wgu[(4 * i + q) % 2]
                    for k in range(KC):
                        col = k * 4 + q
                        kb.idma(wgt[:, k, :], None, mg_rows, IOA(ap=igu[:, i, col:col + 1], axis=0), reads=[igu.r()], writes=[wgt.r(k)])
                        kb.idma(wut[:, k, :], None, mu_rows, IOA(ap=igu[:, i, col:col + 1], axis=0), reads=[igu.r()], writes=[wut.r(k)])

                def load_wd(i):
                    for j in range(28):
                        kb.idma(wd[:, j, :], None, md_rows, IOA(ap=idn[:, i, j:j + 1], axis=0), reads=[idn.r()], writes=[wd.r(j)])

                load_wgu(0, 0)
                load_wgu(0, 1)
                load_wd(0)
                nev = 0
                def fetch_loads(i):
                    for c4 in range(4):
                        s0 = i * 512 + c4 * 128
                        kb.dma("sp", hsb[c4][:], hslot_d.t[s0:s0 + 128, :], reads=sc_res if i == 0 else [], writes=[hsb[c4].r()])

                def fetch_tile(i):
                    hTn = hTt[i % 2]
                    for c4 in range(4):
                        hs = hsb[c4]
                        for k in range(KC):
                            kb.op("pe", lambda e: e.transpose(pbf6[:, k * 128:(k + 1) * 128], hs[:, k * 128:(k + 1) * 128], ident_b[:]),
                                  [hs.r(), ident_b.r()], [PB[6].r()])
                        cp("dve", hTn[:, :, c4 * 128:(c4 + 1) * 128], pbf6[:, :].rearrange("p (a b) -> p a b", b=128), [PB[6].r()], [hTn.r()])

                fetch_loads(0)
                for i in range(NTM):
                    hT_ = hTt[i % 2]
                    fetch_tile(i)
                    for q in range(4):
                        wgt, wut = wgu[(4 * i + q) % 2]
                        for j in range(7):
                            jj = q * 7 + j
                            pg, pu = PB[(j % 2) * 2], PB[(j % 2) * 2 + 1]
                            for k in range(KC):
                                mm(pg[:, :], wgt[:, k, j * 128:(j + 1) * 128], hT_[:, k, :], k == 0, k == KC - 1, [wgt.r(k), hT_.r()], [pg.r()])
                            for k in range(KC):
                                mm(pu[:, :], wut[:, k, j * 128:(j + 1) * 128], hT_[:, k, :], k == 0, k == KC - 1, [wut.r(k), hT_.r()], [pu.r()])
                            sg = sgb[j % 2]
                            act(sg[:], pg[:, :], AF.Silu, [pg.r()], [sg.r()])
                            tt("dve", a_[:, jj, :], pu[:, :], sg[:], ALU.mult, [pu.r(), sg.r()], [a_.r(jj)])
                        if q < 2:
                            load_wgu(i, q + 2)
                        elif i + 1 < NTM:
                            load_wgu(i + 1, q - 2)
                    if i + 1 < NTM:
                        fetch_loads(i + 1)
                    for sc in range(4):
                        yt = ytb[sc % 2]
                        for fh in range(2):
                            ps = PB[4 + fh]
                            for jj in range(28):
                                mm(ps[:, :], a_[:, jj, sc * 128:(sc + 1) * 128], wd[:, jj, fh * 512:(fh + 1) * 512], jj == 0, jj == 27,
                                   [a_.r(jj), wd.r(jj)], [ps.r()])
                            cp("act" if nev % 2 == 0 else "dve", yt[:, fh * 512:(fh + 1) * 512], ps[:, :], [ps.r()], [yt.r()])
                            nev += 1
                        s0 = i * 512 + sc * 128
                        kb.dma("sp", yslot_d.t[s0:s0 + 128, :], yt[:], reads=[yt.r()], writes=[yslot_d.r((i, sc))])
                    if i + 1 < NTM:
                        load_wd(i + 1)
            with kb.scope():
                ys_res = [yslot_d.r((i, sc)) for i in range(NTM) for sc in range(4)]
                NB = 6
                ehb = [kb.sb("ehb", [128, D], F32) for _ in range(NB)]
                elb = [kb.sb("elb", [128, D], F32) for _ in range(NB)]
                xcb = [kb.sb("xcb", [128, D], F32) for _ in range(NB)]
                ocb = [kb.sb("ocb", [128, D], F32) for _ in range(NB)]
                def xload(c):
                    kb.dma("sp", xcb[c % NB][:], xtm_d.t[c * 128:(c + 1) * 128, :], reads=[xtm_d.r(c)], writes=[xcb[c % NB].r()])

                for c in range(NB):
                    xload(c)
                for c in range(32):
                    eh, el, xc, oc_ = ehb[c % NB], elb[c % NB], xcb[c % NB], ocb[c % NB]
                    kb.idma(eh[:], None, yslot_d.t[:, :], IOA(ap=idx_hi[:, c:c + 1], axis=0), reads=[idx_hi.r()] + (ys_res if c == 0 else []), writes=[eh.r()])
                    kb.idma(el[:], None, yslot_d.t[:, :], IOA(ap=idx_lo[:, c:c + 1], axis=0), reads=[idx_lo.r()], writes=[el.r()])
                    act(eh[:], eh[:], AF.Copy, [eh.r(), chi.r()], [eh.r()], scale=chi[:, c:c + 1])
                    stt(el[:], el[:], clo[:, c:c + 1], eh[:], ALU.mult, ALU.add, [el.r(), clo.r(), eh.r()], [el.r()])
                    tt("dve", el[:], el[:], g5b[:], ALU.mult, [el.r(), g5b.r()], [el.r()])
                    tt("dve", oc_[:], el[:], xc[:], ALU.add, [el.r(), xc.r()], [oc_.r()])
                    kb.dma("sp", out_d.t[c * 128:(c + 1) * 128, :], oc_[:], reads=[oc_.r()], writes=[out_d.r(c)])
                    if c + NB < 32:
                        xload(c + NB)

    marks = []

    def mark(name):
        marks.append((name, dict((k, v) for k, v in kb.cnt.items() if isinstance(k, str))))

    kb.marks = marks

    def layer(l):
        last = (l == 1)
        ntile_res = 8 if last else 9
        mark("mod%d" % l)
        mod_phase(l)
        dbg_dump("aff%d" % l, aff[:], [128, 6, 8, 2], F32)
        with kb.scope():
            ycatT = kb.sb("ycatT", [128, 8, NTOK], BF16)
            ztc = kb.sb("ztc", [128, 2, 384], BF16)
            ztm = Tl(ycatT.t[:, 0:3, :].rearrange("p k t -> p (k t)")[:, 0:32 * 384].rearrange("p (a b) -> p a b", b=384))
            with kb.scope():
                hT = kb.sb("hT", [128, 8, NTOK], BF16)
                mark("normA%d" % l)
                normA_phase(l, hT)
                dbg_dump("hT%d" % l, hT[:], [128, 8, NTOK], BF16)
                if stop == "normA":
                    return True
                mark("attn%d" % l)
                attn_groups(l, hT, ycatT)
                if stop == "proj":
                    return True
                dbg_dump("yatt%d" % l, ycatT[:], [128, 8, NTOK], BF16)
                if stop == "attn":
                    return True
                mark("hyproj%d" % l)
                hyena_proj(l, hT, ztm, ztc)
            dbg_dump("ztm%d" % l, ztm[:], [128, 32, 384], BF16)
            mark("hydft%d" % l)
            hyena_dft(l, S, ztm, ycatT, 0)
            mark("hydftc%d" % l)
            if not last:
                hyena_dft(l, LC, ztc, ycatT, S)
            dbg_dump("ycat_raw%d" % l, ycatT[:], [128, 8, NTOK], BF16)
            mark("gnorm%d" % l)
            group_norm(l, ycatT, ntile_res)
            dbg_dump("ycat%d" % l, ycatT[:], [128, 8, NTOK], BF16)
            mark("wout%d" % l)
            wout_phase(l, ycatT, ntile_res, moe=last)
        dbg_dump("xmix%d" % l, xT_d.t[:, :], [D, NTOK], F32)
        if not last:
            dbg_dump("h2T%d" % l, h2_d.t[:, :, :], [128, 8, NTOK], BF16)
        if stop == "mix%d" % l:
            return True
        if not last:
            fg = fg_d.t[0].rearrange("(k p) n -> p k n", p=128)
            fu = fu_d.t[0].rearrange("(k p) n -> p k n", p=128)
            passes = [(fg, fu, fd_d.t[0], ff0, nff, None) for (ff0, nff) in ((0, 6), (6, 6), (12, 5), (17, 5))]
            mark("ffn%d" % l)
            ffn_phase(l, ntile_res, passes)
            dbg_dump("xffn%d" % l, xT_d.t[:, :], [D, NTOK], F32)
        else:
            mark("moe")
            moe_sparse_phase()
            mark("end")
        return stop == "ffn%d" % l

    for l in range(nlayers):
        if layer(l):
            break
    kb.finish()
    es.close()
    return nc, din, dbg_d, kb


def make_in_maps(inputs):
    f = lambda a: np.ascontiguousarray(np.asarray(a, np.float32))
    cst = _consts()
    sh = {}
    sh["w_mod"] = f(inputs["w_mod"])
    sh["b_mod_pk"] = np.stack([_pk(inputs["b_mod"][l], 48) for l in range(2)])
    sh["norm_pk"] = np.stack([np.concatenate([_pk(inputs["norm_mix"][l], 8), _pk(inputs["norm_ffn"][l], 8)], 1) for l in range(2)])
    sh["w_in"] = f(inputs["w_in"])
    cw = np.asarray(inputs["hy_conv_w"], np.float32)
    cbias = np.asarray(inputs["hy_conv_b"], np.float32)
    conv = []
    for l in range(2):
        arr = np.stack([_pk(cw[l, 0], 9), _pk(cw[l, 1], 9), _pk(cw[l, 2], 9), _pk(cbias[l], 9)], axis=2)
        conv.append(arr.reshape(128, 36))
    sh["conv_pk"] = np.stack(conv)
    sh["hy_w1"] = f(inputs["hy_w1"])
    sh["hy_vec"] = np.stack([np.stack([inputs["hy_freq"][l], inputs["hy_b1"][l], inputs["hy_b2"][l, 0], inputs["hy_b2"][l, 1]], 1) for l in range(2)]).astype(np.float32)
    sh["hy_w2"] = f(inputs["hy_w2"])
    sh["hy_w3"] = f(inputs["hy_w3"])
    sh["hy_skip"] = f(inputs["hy_skip"]).reshape(2, 1, 384)
    sh["qkn_pk"] = np.stack([np.stack([np.tile(np.asarray(inputs[k][l], np.float32), 2) for k in ("na_q_norm", "na_k_norm", "ga_q_norm", "ga_k_norm")], 1)
                             for l in range(2)])
    rpb = np.asarray(inputs["na_rpb"], np.float32)
    kc = np.arange(64)[:, None]
    qc = np.arange(64)[None, :]
    dc = np.clip(kc - qc, -15, 15) + 15
    sh["rpbT"] = np.ascontiguousarray(rpb[:, :, :, dc].transpose(0, 3, 1, 2, 4).reshape(2, 64, 90, 64))
    sh["onorm_pk"] = np.stack([_pk(np.concatenate([inputs["out_norm_hy"][l], inputs["out_norm_na"][l], inputs["out_norm_ga"][l]]), 8) for l in range(2)])
    sh["w_out"] = f(inputs["w_out"])
    for k in ("ffn_w_gate", "ffn_w_up", "ffn_w_down", "moe_router", "moe_w_gate", "moe_w_up", "moe_w_down"):
        sh[k] = f(inputs[k])
    for k, v in cst.items():
        sh["k_" + k] = v
    maps = []
    x = np.asarray(inputs["x"], np.float32)
    c = np.asarray(inputs["c"], np.float32)
    ctx = np.asarray(inputs["ctx"], np.float32)
    cctx = _pk(inputs["c_ctx"], 8)
    for b in range(8):
        m = dict(sh)
        m["x"] = np.ascontiguousarray(x[b])
        m["ctx"] = np.ascontiguousarray(ctx[b])
        m["c_pk"] = np.ascontiguousarray(np.concatenate([_pk(c[b], 8), cctx], 1))
        maps.append(m)
    return maps


_PROG = {}


def kernel(**inputs):
    if "p" not in _PROG:
        _PROG["p"] = build_program()
    nc, din, dbg_d, kb = _PROG["p"]
    maps = make_in_maps(inputs)
    res = run_bass_kernel_spmd(nc, maps, core_ids=list(range(8)))
    out = np.stack([np.asarray(res.results[b]["out"], np.float32) for b in range(8)])
    return out
```
